# Optimizing a Trainium2 kernel written in Bass

```python
import jax, jax.numpy as jnp
from jax import lax
import numpy as np

D_MODEL = 1024
BATCH = 8
SEQ = 4096
DEPTH = 1

LRU_WIDTH = D_MODEL
LRU_BLOCKS = 16
LRU_BLOCK = LRU_WIDTH // LRU_BLOCKS
CONV_WIDTH = 4
LRU_C = 8.0
ATT_HEADS = 8
ATT_HEAD_DIM = D_MODEL // ATT_HEADS
ATT_KV_HEADS = 2
ATT_GROUP = ATT_HEADS // ATT_KV_HEADS
IDX_HEADS = 8
IDX_DIM = 64
TOPK_MAX = 256
Q_BLOCK = 128
MEM_HEADS = 4
MEM_HEAD_DIM = D_MODEL // MEM_HEADS
N_BRANCH = 3
PEER_HEADS = 8
PEER_KEYS = 128
PEER_EXPERTS = PEER_KEYS * PEER_KEYS
PEER_QDIM = 256
PEER_SUBDIM = PEER_QDIM // 2
PEER_TOPK = 16
PEER_CHUNK = 128
ROPE_THETA = 10000.0
EPS = 1e-6

SPLIT_SIZES = (LRU_WIDTH, LRU_WIDTH,
               ATT_HEADS * ATT_HEAD_DIM, ATT_KV_HEADS * ATT_HEAD_DIM, ATT_KV_HEADS * ATT_HEAD_DIM,
               IDX_HEADS * IDX_DIM, IDX_DIM, IDX_HEADS,
               MEM_HEADS * MEM_HEAD_DIM,
               N_BRANCH * D_MODEL)
IN_WIDTH = sum(SPLIT_SIZES)
SPLIT_POINTS = tuple(sum(SPLIT_SIZES[:i + 1]) for i in range(len(SPLIT_SIZES) - 1))

kernel_name = "hybrid_rglru_dsa_mem_peer_block"


def rmsnorm(x, g):
    xf = x.astype(jnp.float32)
    y = xf * lax.rsqrt(jnp.mean(xf * xf, axis=-1, keepdims=True) + EPS)
    return (y * g.astype(jnp.float32)).astype(x.dtype)


def rope(x, pos):
    half = x.shape[-1] // 2
    freq = ROPE_THETA ** (-jnp.arange(half, dtype=jnp.float32) / half)
    ang = pos.astype(jnp.float32)[:, None] * freq[None, :]
    cos = jnp.cos(ang)[None, :, None, :]
    sin = jnp.sin(ang)[None, :, None, :]
    xf = x.astype(jnp.float32)
    x1, x2 = xf[..., :half], xf[..., half:]
    return jnp.concatenate([x1 * cos - x2 * sin, x2 * cos + x1 * sin], axis=-1).astype(x.dtype)


def causal_depthwise_conv(x, w, b):
    y = lax.conv_general_dilated(x, w[:, None, :].astype(x.dtype), window_strides=(1,),
                                 padding=[(CONV_WIDTH - 1, 0)],
                                 dimension_numbers=('NWC', 'WIO', 'NWC'),
                                 feature_group_count=x.shape[-1])
    return y + b.astype(x.dtype)


def rg_lru(x, w_a, b_a, w_i, b_i, lam):
    B, S, C = x.shape
    xb = x.reshape(B, S, LRU_BLOCKS, LRU_BLOCK)
    r = jax.nn.sigmoid(jnp.einsum('bsnc,ncd->bsnd', xb, w_a) + b_a).reshape(B, S, C)
    i = jax.nn.sigmoid(jnp.einsum('bsnc,ncd->bsnd', xb, w_i) + b_i).reshape(B, S, C)
    log_a = -LRU_C * r.astype(jnp.float32) * jax.nn.softplus(-lam.astype(jnp.float32))
    a = jnp.exp(log_a)
    mult = jnp.sqrt(-jnp.expm1(2.0 * log_a))
    u = mult * (i * x).astype(jnp.float32)

    def combine(left, right):
        a1, b1 = left
        a2, b2 = right
        return a1 * a2, a2 * b1 + b2

    _, h = lax.associative_scan(combine, (a, u), axis=1)
    return h.astype(x.dtype)


def dsa_attention(q, k, v, qi, ki, wi):
    B, S = q.shape[0], q.shape[1]
    topk = min(TOPK_MAX, S // 4)
    nb = S // Q_BLOCK
    kpos = jnp.arange(S)
    qpos = kpos.reshape(nb, Q_BLOCK)
    ki_f = ki.astype(jnp.float32)

    def to_blocks(t):
        return jnp.moveaxis(t.reshape((B, nb, Q_BLOCK) + t.shape[2:]), 1, 0)

    def block(args):
        qb, qib, wib, pos = args
        logits = jnp.einsum('bqhd,bsd->bqhs', qib.astype(jnp.float32), ki_f)
        score = jnp.einsum('bqhs,bqh->bqs', jax.nn.relu(logits), wib.astype(jnp.float32))
        causal = kpos[None, :] <= pos[:, None]
        score = jnp.where(causal[None], score, -jnp.inf)
        _, idx = lax.top_k(score, topk)
        valid = idx <= pos[None, :, None]
        kg = jax.vmap(lambda kb, ib: kb[ib])(k, idx)
        vg = jax.vmap(lambda vb, ib: vb[ib])(v, idx)
        qg = qb.reshape(B, Q_BLOCK, ATT_KV_HEADS, ATT_GROUP, ATT_HEAD_DIM)
        s = jnp.einsum('bqgrd,bqkgd->bqgrk', qg, kg).astype(jnp.float32) * (ATT_HEAD_DIM ** -0.5)
        s = jnp.where(valid[:, :, None, None, :], s, -jnp.inf)
        p = jax.nn.softmax(s, axis=-1).astype(vg.dtype)
        o = jnp.einsum('bqgrk,bqkgd->bqgrd', p, vg)
        return o.reshape(B, Q_BLOCK, ATT_HEADS * ATT_HEAD_DIM)

    out = lax.map(block, (to_blocks(q), to_blocks(qi), to_blocks(wi), qpos))
    return jnp.moveaxis(out, 0, 1).reshape(B, S, ATT_HEADS * ATT_HEAD_DIM)


def memory_attention(q, mk, mv):
    B, S = q.shape[0], q.shape[1]
    s = jnp.einsum('bshd,bmhd->bhsm', q, mk).astype(jnp.float32) * (MEM_HEAD_DIM ** -0.5)
    p = jax.nn.softmax(s, axis=-1).astype(mv.dtype)
    o = jnp.einsum('bhsm,bmhd->bshd', p, mv)
    return o.reshape(B, S, MEM_HEADS * MEM_HEAD_DIM)


def peer(x, w_q, sub_keys, u, v):
    B, S, D = x.shape
    q = (x @ w_q).reshape(B, S, PEER_HEADS, 2, PEER_SUBDIM)
    sc = jnp.einsum('bshpc,hpkc->bshpk', q, sub_keys).astype(jnp.float32)
    s1, i1 = lax.top_k(sc[..., 0, :], PEER_TOPK)
    s2, i2 = lax.top_k(sc[..., 1, :], PEER_TOPK)
    cand = (s1[..., :, None] + s2[..., None, :]).reshape(B, S, PEER_HEADS, PEER_TOPK * PEER_TOPK)
    cand_idx = (i1[..., :, None] * PEER_KEYS + i2[..., None, :]).reshape(B, S, PEER_HEADS, PEER_TOPK * PEER_TOPK)
    top_s, sel = lax.top_k(cand, PEER_TOPK)
    eidx = jnp.take_along_axis(cand_idx, sel, axis=-1)
    g = jax.nn.softmax(top_s, axis=-1)
    T = B * S
    nc = T // PEER_CHUNK
    xs = x.reshape(nc, PEER_CHUNK, D)
    es = eidx.reshape(nc, PEER_CHUNK, PEER_HEADS, PEER_TOPK)
    gs = g.reshape(nc, PEER_CHUNK, PEER_HEADS, PEER_TOPK)

    def chunk(args):
        xc, ic, gc = args
        uc = u[ic]
        act = jax.nn.gelu(jnp.einsum('chkd,cd->chk', uc, xc).astype(jnp.float32)) * gc
        vc = v[ic]
        return jnp.einsum('chk,chkd->cd', act.astype(vc.dtype), vc)

    out = lax.map(chunk, (xs, es, gs))
    return out.reshape(B, S, D)


def setup_inputs(seed: int = 0) -> dict:
    key = jax.random.key(seed)
    ks = jax.random.split(key, 26)
    f32 = jnp.float32
    nrm = lambda k, shape, s: jax.random.normal(k, shape, f32) * s
    gain = lambda k, shape: 1.0 + 0.02 * jax.random.normal(k, shape, f32)
    a_c = jax.random.uniform(ks[9], (DEPTH, LRU_WIDTH), f32, minval=0.9, maxval=0.999)
    p = a_c ** (1.0 / LRU_C)
    lam = jnp.log(p) - jnp.log1p(-p)
    return {
        "x": nrm(ks[0], (BATCH, SEQ, D_MODEL), 1.0),
        "mem": nrm(ks[1], (BATCH, 256, D_MODEL), 1.0),
        "norm_mix": gain(ks[2], (DEPTH, D_MODEL)),
        "w_in": nrm(ks[3], (DEPTH, D_MODEL, IN_WIDTH), D_MODEL ** -0.5),
        "conv_w": nrm(ks[4], (DEPTH, CONV_WIDTH, LRU_WIDTH), CONV_WIDTH ** -0.5),
        "conv_b": nrm(ks[5], (DEPTH, LRU_WIDTH), 0.01),
        "lru_wa": nrm(ks[6], (DEPTH, LRU_BLOCKS, LRU_BLOCK, LRU_BLOCK), LRU_BLOCK ** -0.5),
        "lru_ba": nrm(ks[7], (DEPTH, LRU_BLOCKS, LRU_BLOCK), 0.01),
        "lru_wi": nrm(ks[8], (DEPTH, LRU_BLOCKS, LRU_BLOCK, LRU_BLOCK), LRU_BLOCK ** -0.5),
        "lru_bi": nrm(ks[10], (DEPTH, LRU_BLOCKS, LRU_BLOCK), 0.01),
        "lru_lambda": lam,
        "q_norm": gain(ks[11], (DEPTH, ATT_HEAD_DIM)),
        "k_norm": gain(ks[12], (DEPTH, ATT_HEAD_DIM)),
        "idx_k_norm": gain(ks[13], (DEPTH, IDX_DIM)),
        "mem_norm": gain(ks[14], (DEPTH, D_MODEL)),
        "w_mem_kv": nrm(ks[15], (DEPTH, D_MODEL, 2 * MEM_HEADS * MEM_HEAD_DIM), D_MODEL ** -0.5),
        "mem_q_norm": gain(ks[16], (DEPTH, MEM_HEAD_DIM)),
        "mem_k_norm": gain(ks[17], (DEPTH, MEM_HEAD_DIM)),
        "w_out": nrm(ks[18], (DEPTH, D_MODEL, D_MODEL), D_MODEL ** -0.5),
        "norm_ffn": gain(ks[19], (DEPTH, D_MODEL)),
        "peer_wq": nrm(ks[20], (DEPTH, D_MODEL, PEER_HEADS * PEER_QDIM), D_MODEL ** -0.5),
        "peer_subkeys": nrm(ks[21], (DEPTH, PEER_HEADS, 2, PEER_KEYS, PEER_SUBDIM), PEER_SUBDIM ** -0.5),
        "peer_u": nrm(ks[22], (DEPTH, PEER_EXPERTS, D_MODEL), D_MODEL ** -0.5),
        "peer_v": nrm(ks[23], (DEPTH, PEER_EXPERTS, D_MODEL), PEER_HEADS ** -0.5),
    }


def reference(x, mem, norm_mix, w_in, conv_w, conv_b, lru_wa, lru_ba, lru_wi, lru_bi, lru_lambda,
              q_norm, k_norm, idx_k_norm, mem_norm, w_mem_kv, mem_q_norm, mem_k_norm, w_out,
              norm_ffn, peer_wq, peer_subkeys, peer_u, peer_v):
    B, S, D = x.shape
    M = mem.shape[1]
    pos = jnp.arange(S)
    for l in range(DEPTH):
        h = rmsnorm(x, norm_mix[l])
        (lru_x, lru_gate, q, k, v, qi, ki, wi, mq, gates) = jnp.split(h @ w_in[l], SPLIT_POINTS, axis=-1)

        xc = causal_depthwise_conv(lru_x, conv_w[l], conv_b[l])
        y_lru = rg_lru(xc, lru_wa[l], lru_ba[l], lru_wi[l], lru_bi[l], lru_lambda[l]) * jax.nn.gelu(lru_gate)

        q = rope(rmsnorm(q.reshape(B, S, ATT_HEADS, ATT_HEAD_DIM), q_norm[l]), pos)
        k = rope(rmsnorm(k.reshape(B, S, ATT_KV_HEADS, ATT_HEAD_DIM), k_norm[l]), pos)
        v = v.reshape(B, S, ATT_KV_HEADS, ATT_HEAD_DIM)
        qi = rope(qi.reshape(B, S, IDX_HEADS, IDX_DIM), pos) * (IDX_DIM ** -0.5)
        ki = rope(rmsnorm(ki, idx_k_norm[l])[:, :, None, :], pos)[:, :, 0, :]
        wi = wi * (IDX_HEADS ** -0.5)
        y_att = dsa_attention(q, k, v, qi, ki, wi)

        m = rmsnorm(mem, mem_norm[l])
        mk, mv = jnp.split(m @ w_mem_kv[l], 2, axis=-1)
        mk = rmsnorm(mk.reshape(B, M, MEM_HEADS, MEM_HEAD_DIM), mem_k_norm[l])
        mv = mv.reshape(B, M, MEM_HEADS, MEM_HEAD_DIM)
        mq = rmsnorm(mq.reshape(B, S, MEM_HEADS, MEM_HEAD_DIM), mem_q_norm[l])
        y_mem = memory_attention(mq, mk, mv)

        g = jax.nn.sigmoid(gates.reshape(B, S, N_BRANCH, D))
        merged = g[:, :, 0, :] * y_lru + g[:, :, 1, :] * y_att + g[:, :, 2, :] * y_mem
        x = x + merged @ w_out[l]

        x = x + peer(rmsnorm(x, norm_ffn[l]), peer_wq[l], peer_subkeys[l], peer_u[l], peer_v[l])
    return x
```

```python
import numpy as np
import ml_dtypes
from contextlib import ExitStack
import concourse.bass as bass
import concourse.mybir as mybir
from concourse.bass_utils import run_bass_kernel_spmd

F32 = mybir.dt.float32
BF16 = mybir.dt.bfloat16
I32 = mybir.dt.int32
U32 = mybir.dt.uint32
ALU = mybir.AluOpType
AF = mybir.ActivationFunctionType
AX = mybir.AxisListType

D = 1024
EPS = 1e-6
NEG = -1.0e30

C_LX, C_LG, C_Q, C_QS, C_K, C_KS, C_QI, C_QIS, C_KI, C_KIS, C_MQ, C_G = 0, 8, 16, 24, 32, 34, 36, 40, 44, 45, 46, 54
NCH = 78
PV_NMIX, PV_CW, PV_CB, PV_BA, PV_BI, PV_LAM = 0, 8, 40, 48, 56, 64
PV_QN, PV_QNS, PV_KN, PV_KNS, PV_IKN, PV_IKNS = 72, 73, 74, 75, 76, 77
PV_MEMN, PV_MQN, PV_MKN, PV_NFFN = 78, 86, 88, 90
NPV = 98


class Res:
    __slots__ = ("w", "r")

    def __init__(self):
        self.w = None
        self.r = {}


class Tl:
    def __init__(self, t, excl=False):
        self.t = t
        self.res = Res()
        self.excl = excl

    def __getitem__(self, k):
        return self.t[k]


class Ctx:
    NDMA = {"sp": 16, "act": 4, "pool": 32}

    def __init__(self, nc, es):
        self.nc = nc
        self.eng = {"pe": nc.tensor, "dve": nc.vector, "act": nc.scalar, "pool": nc.gpsimd, "sp": nc.sync}
        self.sem = {k: es.enter_context(nc.semaphore("s_" + k)) for k in self.eng}
        self.cnt = {k: 0 for k in self.eng}
        self.waited = {k: {} for k in self.eng}
        self.dsem, self.dcnt, self.dnext = {}, {}, {}
        for k, n in self.NDMA.items():
            self.dsem[k] = [es.enter_context(nc.semaphore("d_%s%d" % (k, i))) for i in range(n)]
            self.dcnt[k] = [0] * n
            self.dnext[k] = 0
        self.nins = 0

    def _wait(self, e, ev):
        if ev is None:
            return
        sem, val = ev
        wd = self.waited[e]
        if wd.get(sem, 0) >= val:
            return
        self.eng[e].wait_ge(sem, val)
        wd[sem] = val

    def _deps(self, e, r, w):
        for x in r:
            self._wait(e, x.res.w)
        for x in w:
            self._wait(e, x.res.w)
            for sem, val in x.res.r.items():
                self._wait(e, (sem, val))

    def _commit(self, ev, r, w):
        for x in r:
            if x.res.r.get(ev[0], 0) < ev[1]:
                x.res.r[ev[0]] = ev[1]
        for x in w:
            x.res.w = ev
            x.res.r = {}

    def op(self, e, fn, r=(), w=()):
        w = list(w) + [x for x in r if x.excl and x not in w]
        self._deps(e, r, w)
        ins = fn(self.eng[e])
        self.cnt[e] += 1
        ins.then_inc(self.sem[e], 1)
        self._commit((self.sem[e], self.cnt[e]), r, w)
        self.nins += 1
        return ins

    def dma(self, e, fn, r=(), w=()):
        self._deps(e, r, w)
        i = self.dnext[e]
        self.dnext[e] = (i + 1) % len(self.dsem[e])
        sem = self.dsem[e][i]
        if self.dcnt[e][i]:
            self._wait(e, (sem, self.dcnt[e][i]))
        ins = fn(self.eng[e])
        self.dcnt[e][i] += 16
        ins.then_inc(sem, 16)
        self._commit((sem, self.dcnt[e][i]), r, w)
        self.nins += 1
        return ins

    def barrier(self, engines=("pe", "dve", "act", "pool", "sp")):
        for e in engines:
            for k in self.eng:
                if k != e and self.cnt[k]:
                    self._wait(e, (self.sem[k], self.cnt[k]))
            for k in self.dsem:
                for i, s in enumerate(self.dsem[k]):
                    if self.dcnt[k][i]:
                        self._wait(e, (s, self.dcnt[k][i]))

    def mm(self, out, lhsT, rhs, start, stop, r, w):
        return self.op("pe", lambda e: e.matmul(out, lhsT, rhs, start=start, stop=stop), r=r, w=w)

    def mmg(self, items, r, w):
        w = list(w) + [x for x in r if x.excl and x not in w]
        self._deps("pe", r, w)
        ins = None
        for (out, lhsT, rhs, st, sp) in items:
            ins = self.eng["pe"].matmul(out, lhsT, rhs, start=st, stop=sp)
            self.nins += 1
        self.cnt["pe"] += 1
        ins.then_inc(self.sem["pe"], 1)
        self._commit((self.sem["pe"], self.cnt["pe"]), r, w)
        return ins

    def trg(self, items, ident, r, w):
        w = list(w) + [x for x in r if x.excl and x not in w]
        self._deps("pe", r, w)
        ins = None
        for (out, in_) in items:
            ins = self.eng["pe"].transpose(out, in_, ident)
            self.nins += 1
        self.cnt["pe"] += 1
        ins.then_inc(self.sem["pe"], 1)
        self._commit((self.sem["pe"], self.cnt["pe"]), r, w)
        return ins

    def tr(self, out, in_, ident, r, w):
        return self.op("pe", lambda e: e.transpose(out, in_, ident), r=r, w=w)

    def act(self, out, in_, func, r, w, bias=None, scale=None, accum_out=None, eng="act"):
        kw = {}
        if bias is not None:
            kw["bias"] = bias
        if scale is not None:
            kw["scale"] = scale
        if accum_out is not None:
            kw["accum_out"] = accum_out
        return self.op(eng, lambda e: e.activation(out=out, in_=in_, func=func, **kw), r=r, w=w)

    def ts(self, eng, out, in0, s1, s2, op0, op1, r, w, accum_out=None):
        if op1 is None:
            return self.op(eng, lambda e: e.tensor_scalar(out=out, in0=in0, scalar1=s1, scalar2=None, op0=op0), r=r, w=w)
        kw = {}
        if accum_out is not None:
            kw["accum_out"] = accum_out
        return self.op(eng, lambda e: e.tensor_scalar(out=out, in0=in0, scalar1=s1, scalar2=s2, op0=op0, op1=op1, **kw),
                       r=r, w=w)

    def tt(self, eng, out, in0, in1, op, r, w):
        return self.op(eng, lambda e: e.tensor_tensor(out=out, in0=in0, in1=in1, op=op), r=r, w=w)

    def stt(self, eng, out, in0, scalar, in1, op0, op1, r, w, accum_out=None):
        kw = {}
        if accum_out is not None:
            kw["accum_out"] = accum_out
        return self.op(eng, lambda e: e.scalar_tensor_tensor(out=out, in0=in0, scalar=scalar, in1=in1, op0=op0, op1=op1, **kw),
                       r=r, w=w)

    def copy(self, eng, out, in_, r, w):
        if eng == "act":
            return self.op("act", lambda e: e.copy(out=out, in_=in_), r=r, w=w)
        return self.op(eng, lambda e: e.tensor_copy(out=out, in_=in_), r=r, w=w)


def build(S, dbg=False):
    NT = S // 128
    BLK = min(512, S)
    NB = S // BLK
    TOPK = min(256, S // 4)
    nc = bass.Bass("TRN2", target_bir_lowering=False)

    def din(name, shape, dt=F32):
        return nc.dram_tensor(name, shape, dt, kind="ExternalInput").ap()

    def dscr(name, shape, dt):
        return nc.dram_tensor(name, shape, dt, kind="ExternalOutput" if dbg else "Internal").ap()

    x = din("x", [S, D])
    mem = din("mem", [256, D])
    w_fm = din("w_fm", [D, NCH * 128])
    w_tm = din("w_tm", [D, 264])
    pvec = din("pvec", [128, NPV])
    wbd = din("wbd", [128, 8 * 2 * 128])
    ropet = din("ropet", [4, 128, S], BF16)
    w_mem_kv = din("w_mem_kv", [D, 2048])
    w_out = din("w_out", [D, D])
    nffn = din("nffn", [1, D])
    nmix = din("nmix", [1, D])
    w_q = din("w_q", [D, 2048])
    skT = din("skT", [128, 16 * 128])
    pu = din("pu", [16384, D])
    pv = din("pv", [16384, D])
    y = nc.dram_tensor("y", [S, D], F32, kind="ExternalOutput").ap()

    s_q = dscr("s_q", [8, 128, S], BF16)
    s_k = dscr("s_k", [2, 128, S], BF16)
    s_v = dscr("s_v", [S, 256], BF16)
    s_wi = dscr("s_wi", [S, 8], F32)
    s_qi = dscr("s_qi", [4, 128, S], BF16)
    s_ki = dscr("s_ki", [128, S], BF16)
    s_mq = dscr("s_mq", [8, 128, S], BF16)
    s_g = dscr("s_g", [24, 128, S], BF16)
    s_ylru = dscr("s_ylru", [8, 128, S], BF16)
    s_yatt = dscr("s_yatt", [8, 128, S], BF16)
    s_x1 = dscr("s_x1", [S, D], F32)
    s_uvb = nc.dram_tensor("s_uvb", [16384, 2 * D], BF16, kind="Internal").ap()

    with ExitStack() as es0:
        es0.enter_context(nc.allow_low_precision("bf16 matmul operands, fp32 accumulation"))
        es0.enter_context(nc.allow_non_contiguous_dma("small strided loads"))
        c = Ctx(nc, es0)

        def sb(es, name, shape, dt):
            return Tl(es.enter_context(nc.sbuf_tensor(name, shape, dt)))

        PS = [Tl(es0.enter_context(nc.psum_tensor("ps%d" % i, [128, 512], F32)), True) for i in range(6)]
        PSB = [Tl(es0.enter_context(nc.psum_tensor("psb%d" % i, [128, 1024], BF16)), True) for i in range(2)]

        ident = sb(es0, "ident", [128, 128], BF16)
        identf = sb(es0, "identf", [128, 128], F32)
        ones = sb(es0, "ones", [128, 128], BF16)
        onesm = sb(es0, "onesm", [128, 3 * 128], BF16)
        pv_t = sb(es0, "pvec_sb", [128, NPV], F32)
        c.op("pool", lambda e: e.memset(identf[:], 0.0), w=[identf])
        c.op("pool", lambda e: e.affine_select(out=identf[:], in_=identf[:], pattern=[[-1, 128]],
                                               compare_op=ALU.not_equal, fill=1.0, base=0, channel_multiplier=1),
             r=[identf], w=[identf])
        c.copy("dve", ident[:], identf[:], r=[identf], w=[ident])
        c.op("dve", lambda e: e.memset(ones[:], 1.0), w=[ones])
        zeros = sb(es0, "zeros", [128, 128], BF16)
        c.op("dve", lambda e: e.memset(zeros[:], 0.0), w=[zeros])
        c.op("dve", lambda e: e.memset(onesm[:, 0:128], 1.0 / 128), w=[onesm])
        c.op("dve", lambda e: e.memset(onesm[:, 128:256], 0.0), w=[onesm])
        c.op("dve", lambda e: e.memset(onesm[0:64, 128:192], 1.0 / 64), w=[onesm])
        c.op("dve", lambda e: e.memset(onesm[64:128, 192:256], 1.0 / 64), w=[onesm])
        c.op("dve", lambda e: e.memset(onesm[:, 256:384], 1.0 / 256), w=[onesm])
        c.dma("sp", lambda e: e.dma_start(out=pv_t[:], in_=pvec), w=[pv_t])
        perm = sb(es0, "perm", [128, 2, 128], BF16)
        c.copy("dve", perm[:, 0, 0:64], ident[:, 64:128], r=[ident], w=[perm])
        c.copy("dve", perm[:, 0, 64:128], ident[:, 0:64], r=[ident], w=[perm])
        for q4 in range(4):
            src = (q4 ^ 1) * 32
            c.copy("dve", perm[:, 1, q4 * 32:(q4 + 1) * 32], ident[:, src:src + 32], r=[ident], w=[perm])


        def load_bf16(dst_tl, dst_ap, src_ap):
            c.dma("pool", lambda e: e.dma_start(out=dst_ap, in_=src_ap), w=[dst_tl])

        def rstd_from_ms(out_ap, ms_ap, r, w):
            c.act(out_ap, ms_ap, AF.Ln, r=r, w=w, bias=epsb[:, 0:1], scale=1.0)
            c.act(out_ap, out_ap, AF.Exp, r=w, w=w, scale=-0.5)

        epsb = sb(es0, "epsb", [128, 2], F32)
        c.op("dve", lambda e: e.memset(epsb[:, 0:1], EPS), w=[epsb])
        c.op("dve", lambda e: e.memset(epsb[:, 1:2], 1.0), w=[epsb])

        def rmsnorm_rows(es, name, src_tl, width):
            junk = sb(es, name + "_junk", [128, width], BF16)
            ss = sb(es, name + "_ss", [128, 1], F32)
            c.act(junk[:], src_tl[:, 0:width], AF.Square, r=[src_tl], w=[junk, ss], accum_out=ss[:, 0:1])
            c.act(ss[:], ss[:], AF.Sqrt, r=[ss, epsb], w=[ss], bias=epsb[:, 0:1], scale=1.0 / width)
            c.op("dve", lambda e: e.reciprocal(out=ss[:], in_=ss[:]), r=[ss], w=[ss])
            return ss

        mkT = sb(es0, "mkT", [128, 8, 256], BF16)
        mv = sb(es0, "mv", [128, 2, D], BF16)
        with ExitStack() as es:
            wkv = sb(es, "wkv", [128, 8, 2048], BF16)
            for kc in range(8):
                load_bf16(wkv, wkv[:, kc, :], w_mem_kv[kc * 128:(kc + 1) * 128, :])
            mT = sb(es, "mT", [128, 8, 256], BF16)
            for mt in range(2):
                mx = sb(es, "mx%d" % mt, [128, D], F32)
                c.dma("sp", lambda e: e.dma_start(out=mx[:], in_=mem[mt * 128:(mt + 1) * 128, :]), w=[mx])
                rs = rmsnorm_rows(es, "mrs%d" % mt, mx, D)
                mh = sb(es, "mh%d" % mt, [128, D], BF16)
                c.act(mh[:], mx[:], AF.Copy, r=[mx, rs], w=[mh], scale=rs[:, 0:1])
                for kc in range(8):
                    pb = PSB[kc % 2]
                    po = pb.t[:, 0:128]
                    c.tr(po, mh[:, kc * 128:(kc + 1) * 128], ident[:], r=[mh, ident], w=[pb])
                    c.ts("dve", mT[:, kc, mt * 128:(mt + 1) * 128], po, pv_t[:, PV_MEMN + kc:PV_MEMN + kc + 1], None,
                         ALU.mult, None, r=[pb, pv_t], w=[mT])
            for mt in range(2):
                mkf = sb(es, "mkf%d" % mt, [128, D], F32)
                for half in range(2):
                    ps = PS[half]
                    for kc in range(8):
                        c.mm(ps[:], mT[:, kc, mt * 128:(mt + 1) * 128], wkv[:, kc, half * 512:(half + 1) * 512],
                             kc == 0, kc == 7, r=[mT, wkv], w=[ps])
                    c.copy("act", mkf[:, half * 512:(half + 1) * 512], ps[:], r=[ps], w=[mkf])
                mkb = sb(es, "mkb%d" % mt, [128, D], BF16)
                for h in range(4):
                    junk = sb(es, "mkj%d_%d" % (mt, h), [128, 256], BF16)
                    ss = sb(es, "mks%d_%d" % (mt, h), [128, 1], F32)
                    c.act(junk[:], mkf[:, h * 256:(h + 1) * 256], AF.Square, r=[mkf], w=[junk, ss], accum_out=ss[:, 0:1])
                    c.act(ss[:], ss[:], AF.Sqrt, r=[ss, epsb], w=[ss], bias=epsb[:, 0:1], scale=1.0 / 256)
                    c.op("dve", lambda e: e.reciprocal(out=ss[:], in_=ss[:]), r=[ss], w=[ss])
                    c.act(mkb[:, h * 256:(h + 1) * 256], mkf[:, h * 256:(h + 1) * 256], AF.Copy, r=[mkf, ss], w=[mkb],
                          scale=ss[:, 0:1])
                for kc in range(8):
                    pb = PSB[kc % 2]
                    po = pb.t[:, 0:128]
                    c.tr(po, mkb[:, kc * 128:(kc + 1) * 128], ident[:], r=[mkb, ident], w=[pb])
                    gcol = PV_MKN + (kc % 2)
                    c.ts("dve", mkT[:, kc, mt * 128:(mt + 1) * 128], po, pv_t[:, gcol:gcol + 1], None,
                         ALU.mult, None, r=[pb, pv_t], w=[mkT])
                for half in range(2):
                    ps = PS[2 + half]
                    for kc in range(8):
                        c.mm(ps[:], mT[:, kc, mt * 128:(mt + 1) * 128], wkv[:, kc, 1024 + half * 512:1024 + (half + 1) * 512],
                             kc == 0, kc == 7, r=[mT, wkv], w=[ps])
                    c.copy("act", mv[:, mt, half * 512:(half + 1) * 512], ps[:], r=[ps], w=[mv])
            c.barrier()

        if dbg:
            d_mkT = nc.dram_tensor("d_mkT", [128, 8 * 256], BF16, kind="ExternalOutput").ap()
            d_mv = nc.dram_tensor("d_mv", [128, 2 * D], BF16, kind="ExternalOutput").ap()
            c.dma("sp", lambda e: e.dma_start(out=d_mkT, in_=mkT[:].rearrange("p a b -> p (a b)")), r=[mkT])
            c.dma("sp", lambda e: e.dma_start(out=d_mv, in_=mv[:].rearrange("p a b -> p (a b)")), r=[mv])

        PHASES = build.phases

        if PHASES >= 1:
          with ExitStack() as es:
            hT = sb(es, "hT", [128, 8, S], BF16)
            NW = 4
            wch = [sb(es, "wch%d" % i, [128, 8, 128], BF16) for i in range(NW)]
            wstate = {"n": 0}

            def load_chunk(ci):
                wt = wch[wstate["n"] % NW]
                wstate["n"] += 1
                load_bf16(wt, wt[:], w_fm[:, ci * 128:(ci + 1) * 128].rearrange("(kc p) n -> p kc n", p=128))
                return wt

            def proj(wt, blk, ps):
                c.mmg([(ps[:, 0:BLK], wt[:, kc, :], hT[:, kc, blk * BLK:(blk + 1) * BLK], kc == 0, kc == 7) for kc in range(8)],
                      r=[wt, hT], w=[ps])

            with ExitStack() as es1:
                wtm = sb(es1, "wtm", [128, 8, 512], BF16)
                c.op("dve", lambda e: e.memset(wtm[:], 0.0), w=[wtm])
                for kc in range(8):
                    load_bf16(wtm, wtm[:, kc, 0:264], w_tm[kc * 128:(kc + 1) * 128, :])
                xr = [sb(es1, "xr%d" % i, [128, D], F32) for i in range(2)]
                hb = [sb(es1, "hb%d" % i, [128, D], BF16) for i in range(2)]
                jk = [sb(es1, "jk%d" % i, [128, D], BF16) for i in range(2)]
                s1 = [sb(es1, "s1_%d" % i, [128, 1], F32) for i in range(2)]
                vo = [sb(es1, "vo%d" % i, [128, 256], BF16) for i in range(2)]
                wo = [sb(es1, "wo%d" % i, [128, 8], F32) for i in range(2)]
                gnm = sb(es1, "gnm", [128, D], F32)
                c.dma("sp", lambda e: e.dma_start(out=gnm[:], in_=nmix.partition_broadcast(128)), w=[gnm])

                def norm_tile(t):
                    b = t % 2
                    c.dma("sp", lambda e: e.dma_start(out=xr[b][:], in_=x[t * 128:(t + 1) * 128, :]), w=[xr[b]])
                    c.act(jk[b][:], xr[b][:], AF.Square, r=[xr[b]], w=[jk[b], s1[b]], accum_out=s1[b][:, 0:1])
                    c.act(s1[b][:], s1[b][:], AF.Sqrt, r=[s1[b], epsb], w=[s1[b]], bias=epsb[:, 0:1], scale=1.0 / D)
                    c.op("dve", lambda e: e.reciprocal(out=s1[b][:], in_=s1[b][:]), r=[s1[b]], w=[s1[b]])
                    c.stt("dve", hb[b][:], xr[b][:], s1[b][:, 0:1], gnm[:], ALU.mult, ALU.mult, r=[xr[b], s1[b], gnm], w=[hb[b]])

                norm_tile(0)
                for t in range(NT):
                    b = t % 2
                    if t + 1 < NT:
                        norm_tile(t + 1)
                    pb = PSB[t % 2]
                    c.trg([(pb.t[:, kc * 128:(kc + 1) * 128], hb[b][:, kc * 128:(kc + 1) * 128]) for kc in range(8)], ident[:],
                          r=[hb[b], ident], w=[pb])
                    c.copy("act", hT[:, :, t * 128:(t + 1) * 128], pb.t[:].rearrange("p (a b) -> p a b", b=128), r=[pb], w=[hT])
                    if "tm" in build.skip:
                        continue
                    ps = PS[t % 2]
                    c.mmg([(ps[:], hT[:, kc, t * 128:(t + 1) * 128], wtm[:, kc, :], kc == 0, kc == 7) for kc in range(8)],
                          r=[hT, wtm], w=[ps])
                    c.copy("act", vo[b][:], ps[:, 0:256], r=[ps], w=[vo[b]])
                    if "wo" not in build.skip:
                        c.ts("dve", wo[b][:], ps[:, 256:264], 8.0 ** -0.5, None, ALU.mult, None, r=[ps] + ([vo[b]] if "ser" in build.skip else []), w=[wo[b]])
                    if "vdma" not in build.skip:
                        c.dma("sp", lambda e: e.dma_start(out=s_v[t * 128:(t + 1) * 128, :], in_=vo[b][:]), r=[vo[b]])
                    if "wi" not in build.skip:
                        c.dma("sp", lambda e: e.dma_start(out=s_wi[t * 128:(t + 1) * 128, :], in_=wo[b][:]), r=[wo[b]])
                c.barrier()

            ring = {}

            def rt(esx, name, shape, dt, n=2):
                if name not in ring:
                    ring[name] = [[sb(esx, "%s_%d" % (name, i), shape, dt) for i in range(n)], 0]
                lst = ring[name]
                tl = lst[0][lst[1] % n]
                lst[1] += 1
                return tl

            if PHASES >= 2:
              with ExitStack() as es2:
                rope = sb(es2, "rope", [128, 4, S], BF16)
                for i in range(4):
                    c.dma("sp", lambda e: e.dma_start(out=rope[:, i, :], in_=ropet[i]), w=[rope])
                nrc = {"n": 0}

                def norm_rope(wts, blk, out_dram, ti, gcol, gscol, mean_off, extra_scale):
                    wa = wts[0]
                    par = nrc["n"] % 2
                    nrc["n"] += 1
                    pa, pbk = PS[2 * par], PS[2 * par + 1]
                    proj(wa, blk, pa)
                    qsb = rt(es2, "nr_qsb", [128, BLK], BF16)
                    c.copy("act", qsb[:], pa[:, 0:BLK], r=[pa], w=[qsb])
                    c.mm(pbk[:, 0:BLK], perm[:, 0 if ti == 0 else 1, :], qsb[:], True, True, r=[perm, qsb], w=[pbk])
                    sl = slice(blk * BLK, (blk + 1) * BLK)
                    t1 = rt(es2, "nr_t1", [128, BLK], F32)
                    t2 = rt(es2, "nr_t2", [128, BLK], F32)
                    ob = rt(es2, "nr_ob", [128, BLK], BF16)
                    if gcol is not None:
                        c.stt("dve", t1[:], pa[:, 0:BLK], pv_t[:, gcol:gcol + 1], rope[:, ti, sl], ALU.mult, ALU.mult,
                              r=[pa, pv_t, rope], w=[t1])
                        c.stt("dve", t2[:], pbk[:, 0:BLK], pv_t[:, gscol:gscol + 1], rope[:, ti + 1, sl], ALU.mult, ALU.mult,
                              r=[pbk, pv_t, rope], w=[t2])
                    else:
                        c.tt("dve", t1[:], pa[:, 0:BLK], rope[:, ti, sl], ALU.mult, r=[pa, rope], w=[t1])
                        c.tt("dve", t2[:], pbk[:, 0:BLK], rope[:, ti + 1, sl], ALU.mult, r=[pbk, rope], w=[t2])
                    if mean_off is not None:
                        sq = rt(es2, "nr_sq", [128, BLK], BF16)
                        c.act(sq[:], pa[:, 0:BLK], AF.Square, r=[pa], w=[sq])
                        pm = PS[4 + par]
                        c.mm(pm[:, 0:BLK], onesm[:, mean_off:mean_off + 128], sq[:], True, True, r=[onesm, sq], w=[pm])
                        rs = rt(es2, "nr_rs", [128, BLK], F32)
                        rstd_from_ms(rs[:], pm[:, 0:BLK], r=[pm, epsb], w=[rs])
                        c.tt("dve", t1[:], t1[:], t2[:], ALU.add, r=[t1, t2], w=[t1])
                        c.tt("dve", ob[:], t1[:], rs[:], ALU.mult, r=[t1, rs], w=[ob])
                    else:
                        c.stt("dve", ob[:], t1[:], extra_scale, t2[:], ALU.mult, ALU.mult, r=[t1, t2], w=[ob]) if False else None
                        c.tt("dve", t1[:], t1[:], t2[:], ALU.add, r=[t1, t2], w=[t1])
                        c.act(ob[:], t1[:], AF.Copy, r=[t1], w=[ob], scale=extra_scale)
                    c.dma("sp", lambda e: e.dma_start(out=out_dram[:, sl], in_=ob[:]), r=[ob])

                def mq_head(wts, hm, blk):
                    wa, wb = wts
                    par = nrc["n"] % 2
                    nrc["n"] += 1
                    pa, pbk, pm = PS[2 * par], PS[2 * par + 1], PS[4 + par]
                    proj(wa, blk, pa)
                    proj(wb, blk, pbk)
                    sl = slice(blk * BLK, (blk + 1) * BLK)
                    sqa = rt(es2, "nr_sq", [128, BLK], BF16)
                    sqb = rt(es2, "nr_sq", [128, BLK], BF16)
                    c.act(sqa[:], pa[:, 0:BLK], AF.Square, r=[pa], w=[sqa])
                    c.act(sqb[:], pbk[:, 0:BLK], AF.Square, r=[pbk], w=[sqb])
                    c.mm(pm[:, 0:BLK], onesm[:, 256:384], sqa[:], True, False, r=[onesm, sqa], w=[pm])
                    c.mm(pm[:, 0:BLK], onesm[:, 256:384], sqb[:], False, True, r=[onesm, sqb], w=[pm])
                    rs = rt(es2, "nr_rs", [128, BLK], F32)
                    rstd_from_ms(rs[:], pm[:, 0:BLK], r=[pm, epsb], w=[rs])
                    for cc, pp in ((0, pa), (1, pbk)):
                        ob = rt(es2, "nr_ob", [128, BLK], BF16)
                        c.stt("dve", ob[:], pp[:, 0:BLK], pv_t[:, PV_MQN + cc:PV_MQN + cc + 1], rs[:], ALU.mult, ALU.mult,
                              r=[pp, pv_t, rs], w=[ob])
                        c.dma("sp", lambda e: e.dma_start(out=s_mq[2 * hm + cc][:, sl], in_=ob[:]), r=[ob])

                def gate_chunk(wts, gi, blk):
                    wa = wts[0]
                    par = nrc["n"] % 4
                    nrc["n"] += 1
                    pa = PS[par]
                    proj(wa, blk, pa)
                    sl = slice(blk * BLK, (blk + 1) * BLK)
                    ob = rt(es2, "nr_ob", [128, BLK], BF16)
                    c.act(ob[:], pa[:, 0:BLK], AF.Sigmoid, r=[pa], w=[ob])
                    c.dma("sp", lambda e: e.dma_start(out=s_g[gi][:, sl], in_=ob[:]), r=[ob])

                jobs = []
                for h in range(8):
                    jobs.append(([C_Q + h], lambda w_, blk, h=h: norm_rope(w_, blk, s_q[h], 0, PV_QN, PV_QNS, 0, None)))
                for g in range(2):
                    jobs.append(([C_K + g], lambda w_, blk, g=g: norm_rope(w_, blk, s_k[g], 0, PV_KN, PV_KNS, 0, None)))
                for p in range(4):
                    jobs.append(([C_QI + p], lambda w_, blk, p=p: norm_rope(w_, blk, s_qi[p], 2, None, None, None, 64.0 ** -0.5)))
                jobs.append(([C_KI], lambda w_, blk: norm_rope(w_, blk, s_ki, 2, PV_IKN, PV_IKNS, 128, None)))
                for hm in range(4):
                    jobs.append(([C_MQ + 2 * hm, C_MQ + 2 * hm + 1], lambda w_, blk, hm=hm: mq_head(w_, hm, blk)))
                for gi in range(24):
                    jobs.append(([C_G + gi], lambda w_, blk, gi=gi: gate_chunk(w_, gi, blk)))
                pending = [load_chunk(ci) for ci in jobs[0][0]]
                for ji, (chs, fn) in enumerate(jobs):
                    wts = pending
                    if ji + 1 < len(jobs):
                        pending = [load_chunk(ci) for ci in jobs[ji + 1][0]]
                    for blk in range(NB):
                        fn(wts, blk)
                c.barrier()

            if PHASES >= 2:
              with ExitStack() as es3:
                TCH = min(S, 1024)
                NTC = S // TCH
                NSB = TCH // BLK
                wbd_t = sb(es3, "wbd_t", [128, 8 * 2 * 128], BF16)
                load_bf16(wbd_t, wbd_t[:], wbd)
                nsp = sb(es3, "nsp", [128, 24], F32)
                c.act(nsp[:, 0:8], pv_t[:, PV_LAM:PV_LAM + 8], AF.Exp, r=[pv_t], w=[nsp], scale=-1.0)
                c.act(nsp[:, 0:8], nsp[:, 0:8], AF.Ln, r=[nsp, epsb], w=[nsp], bias=epsb[:, 1:2], scale=1.0)
                c.ts("dve", nsp[:, 8:16], nsp[:, 0:8], -8.0, None, ALU.mult, None, r=[nsp], w=[nsp])
                c.ts("dve", nsp[:, 16:24], nsp[:, 0:8], -16.0, None, ALU.mult, None, r=[nsp], w=[nsp])
                lx = sb(es3, "lx", [128, S + 3], F32)
                lruL = [[sb(es3, "%s%d" % (nm, k_), [128, TCH], dt_) for k_ in range(2)]
                        for nm, dt_ in (("xc", F32), ("xcb", BF16), ("rr", F32), ("ii", F32), ("a2", F32), ("hh", F32),
                                        ("gx", F32), ("gt", F32), ("yb", BF16))]
                lcnt = 0
                hprev = sb(es3, "hprev", [128, 1], F32)
                c.op("dve", lambda e: e.memset(lx[:, 0:3], 0.0), w=[lx])
                pend = (load_chunk(C_LX), load_chunk(C_LG))
                for ci in range(8):
                    wa, wg = pend
                    if ci + 1 < 8:
                        pend = (load_chunk(C_LX + ci + 1), load_chunk(C_LG + ci + 1))
                    for blk in range(NB):
                        ps = PS[blk % 2]
                        proj(wa, blk, ps)
                        c.copy("act", lx[:, 3 + blk * BLK:3 + (blk + 1) * BLK], ps[:, 0:BLK], r=[ps], w=[lx])
                    for tc in range(NTC):
                        o = tc * TCH
                        xc, xcb, rr, ii, a2, hh, gx, gt, yb = [L_[lcnt % 2] for L_ in lruL]
                        lcnt += 1
                        cw = lambda j: pv_t[:, PV_CW + 8 * j + ci:PV_CW + 8 * j + ci + 1]
                        c.ts("dve", xc[:], lx[:, o:o + TCH], cw(0), pv_t[:, PV_CB + ci:PV_CB + ci + 1], ALU.mult, ALU.add,
                             r=[lx, pv_t], w=[xc])
                        for j in range(1, 4):
                            c.stt("dve", xc[:], lx[:, o + j:o + j + TCH], cw(j), xc[:], ALU.mult, ALU.add,
                                  r=[lx, pv_t, xc], w=[xc])
                        c.copy("act", xcb[:], xc[:], r=[xc], w=[xcb])
                        for sbk in range(NSB):
                            ss_ = slice(sbk * BLK, (sbk + 1) * BLK)
                            pa, pi = PS[2 + (sbk % 2) * 2], PS[3 + (sbk % 2) * 2]
                            c.mm(pa[:, 0:BLK], wbd_t[:, (ci * 2) * 128:(ci * 2 + 1) * 128], xcb[:, ss_], True, True, r=[wbd_t, xcb], w=[pa])
                            c.mm(pi[:, 0:BLK], wbd_t[:, (ci * 2 + 1) * 128:(ci * 2 + 2) * 128], xcb[:, ss_], True, True, r=[wbd_t, xcb], w=[pi])
                            c.act(rr[:, ss_], pa[:, 0:BLK], AF.Sigmoid, r=[pa, pv_t], w=[rr], bias=pv_t[:, PV_BA + ci:PV_BA + ci + 1], scale=1.0)
                            c.act(ii[:, ss_], pi[:, 0:BLK], AF.Sigmoid, r=[pi, pv_t], w=[ii], bias=pv_t[:, PV_BI + ci:PV_BI + ci + 1], scale=1.0)
                        c.act(a2[:], rr[:], AF.Exp, r=[rr, nsp], w=[a2], scale=nsp[:, 16 + ci:17 + ci])
                        c.act(rr[:], rr[:], AF.Exp, r=[rr, nsp], w=[rr], scale=nsp[:, 8 + ci:9 + ci])
                        c.act(a2[:], a2[:], AF.Sqrt, r=[a2, epsb], w=[a2], bias=epsb[:, 1:2], scale=-1.0)
                        c.tt("dve", ii[:], ii[:], xc[:], ALU.mult, r=[ii, xc], w=[ii])
                        c.tt("dve", ii[:], ii[:], a2[:], ALU.mult, r=[ii, a2], w=[ii])
                        if tc == 0:
                            c.op("dve", lambda e: e.tensor_tensor_scan(out=hh[:], data0=rr[:], data1=ii[:], initial=0.0,
                                                                       op0=ALU.mult, op1=ALU.add), r=[rr, ii], w=[hh])
                        else:
                            c.op("dve", lambda e: e.tensor_tensor_scan(out=hh[:], data0=rr[:], data1=ii[:], initial=hprev[:, 0:1],
                                                                       op0=ALU.mult, op1=ALU.add), r=[rr, ii, hprev], w=[hh])
                        if tc + 1 < NTC:
                            c.copy("dve", hprev[:], hh[:, TCH - 1:TCH], r=[hh], w=[hprev])
                        for sbk in range(NSB):
                            ss_ = slice(sbk * BLK, (sbk + 1) * BLK)
                            ps = PS[sbk % 2]
                            proj(wg, tc * NSB + sbk, ps)
                            c.copy("act", gx[:, ss_], ps[:, 0:BLK], r=[ps], w=[gx])
                            c.act(gt[:, ss_], ps[:, 0:BLK], AF.Square, r=[ps], w=[gt])
                        c.ts("dve", gt[:], gt[:], 0.044715, 1.0, ALU.mult, ALU.add, r=[gt], w=[gt])
                        c.tt("dve", gt[:], gt[:], gx[:], ALU.mult, r=[gt, gx], w=[gt])
                        c.act(gt[:], gt[:], AF.Sigmoid, r=[gt], w=[gt], scale=1.5957691216057308)
                        c.tt("dve", gt[:], gt[:], gx[:], ALU.mult, r=[gt, gx], w=[gt])
                        c.tt("dve", yb[:], gt[:], hh[:], ALU.mult, r=[gt, hh], w=[yb])
                        c.dma("sp", lambda e: e.dma_start(out=s_ylru[ci][:, o:o + TCH], in_=yb[:]), r=[yb])
                c.barrier()

        if PHASES >= 3:
          with ExitStack() as es:
            NIT = 14
            for i in range(16 if build.phases >= 5 else 0):
                c.dma("pool", lambda e: e.dma_start(out=s_uvb[i * 1024:(i + 1) * 1024, 0:D], in_=pu[i * 1024:(i + 1) * 1024, :]))
                c.dma("pool", lambda e: e.dma_start(out=s_uvb[i * 1024:(i + 1) * 1024, D:2 * D], in_=pv[i * 1024:(i + 1) * 1024, :]))
            kT = sb(es, "kT", [128, 2, S], BF16)
            vt = sb(es, "vt", [128, NT, 256], BF16)
            ki2 = sb(es, "ki2", [128, S], BF16)
            wi_t = sb(es, "wi_t", [128, NT, 8], F32)
            c.dma("sp", lambda e: e.dma_start(out=kT[:], in_=s_k.rearrange("g p t -> p g t")), w=[kT])
            c.dma("sp", lambda e: e.dma_start(out=vt[:], in_=s_v.rearrange("(t p) d -> p t d", p=128)), w=[vt])
            c.dma("sp", lambda e: e.dma_start(out=ki2[:], in_=s_ki), w=[ki2])
            c.dma("sp", lambda e: e.dma_start(out=wi_t[:], in_=s_wi.rearrange("(t p) h -> p t h", p=128)), w=[wi_t])
            cmask = sb(es, "cmask", [128, 128], F32)
            c.op("pool", lambda e: e.memset(cmask[:], 0.0), w=[cmask])
            c.op("pool", lambda e: e.affine_select(out=cmask[:], in_=cmask[:], pattern=[[-1, 128]], compare_op=ALU.is_ge,
                                                   fill=NEG, base=0, channel_multiplier=1), r=[cmask], w=[cmask])
            pw2 = sb(es, "pw2", [128, NIT], F32)
            for it in range(NIT):
                c.op("pool", lambda e: e.memset(pw2[:, it:it + 1], 2.0 ** -(it + 1)), w=[pw2])
            scd = [sb(es, "sc%d" % i, [128, S], F32) for i in range(4)]
            maskd = [sb(es, "maskb%d" % i, [128, S], BF16) for i in range(2)]
            mTd = [sb(es, "mT_sb%d" % i, [128, NT, 128], BF16) for i in range(4)]
            qT = [sb(es, "qT%d" % i, [128, 8, 128], BF16) for i in range(4)]
            qiT = [sb(es, "qiT%d" % i, [128, 4, 128], BF16) for i in range(4)]
            rl = [sb(es, "rl%d" % i, [128, 512], F32) for i in range(4)]
            Eb = [sb(es, "Eb%d" % i, [128, 512], BF16) for i in range(3)]
            Pb = [sb(es, "Pb%d" % i, [128, 512], BF16) for i in range(3)]
            std = [sb(es, "st%d" % i, [128, 8], F32) for i in range(4)]
            wkd = [sb(es, "wk%d" % i, [128, NIT], F32) for i in range(4)]
            fin_o = [sb(es, "fin_o%d" % i, [128, 512], F32) for i in range(2)]
            fin_d = [sb(es, "fin_d%d" % i, [128, 512], F32) for i in range(2)]
            yat = [sb(es, "yat%d" % i, [128, 512], BF16) for i in range(2)]
            scale_att = 128.0 ** -0.5
            cntE = {"n": 0, "f": 0}

            def idx_steps(tiles):
                T_ = []
                for i in tiles:
                    d = dict(i=i, qs=slice(i * 128, (i + 1) * 128), nk=128 * (i + 1), sc=scd[i % 4], st=std[i % 4], wk=wkd[i % 4],
                             maskb=maskd[i % 2], mT=mTd[i % 4], q=qT[i % 4], qi=qiT[i % 4], junk=maskd[i % 2], ps=PS[i % 2],
                             rl=(rl[(i % 2) * 2], rl[(i % 2) * 2 + 1]))
                    T_.append(d)
                    c.dma("sp", lambda e: e.dma_start(out=d["q"][:], in_=s_q[:, :, d["qs"]].rearrange("h p t -> p h t")), w=[d["q"]])
                    c.dma("sp", lambda e: e.dma_start(out=d["qi"][:], in_=s_qi[:, :, d["qs"]].rearrange("h p t -> p h t")), w=[d["qi"]])
                nkb_max = max((d["nk"] + 511) // 512 for d in T_)
                for kb in range(nkb_max):
                    for h in range(8):
                        for d in T_:
                            if kb * 512 >= d["nk"]:
                                continue
                            ksz = min(512, d["nk"] - kb * 512)
                            ks = slice(kb * 512, kb * 512 + ksz)
                            pp, sc, i = d["ps"], d["sc"], d["i"]
                            lo_, hi_ = (h % 2) * 64, (h % 2) * 64 + 64
                            c.mm(pp[:, 0:ksz], d["qi"][lo_:hi_, h // 2, :], ki2[lo_:hi_, ks], True, True, r=[d["qi"], ki2], w=[pp])
                            r_ = d["rl"][h % 2]
                            c.act(r_[:, 0:ksz], pp[:, 0:ksz], AF.Relu, r=[pp], w=[r_])
                            if h == 0:
                                c.ts("dve", sc[:, ks], r_[:, 0:ksz], wi_t[:, i, 0:1], None, ALU.mult, None, r=[r_, wi_t], w=[sc])
                            else:
                                c.stt("dve", sc[:, ks], r_[:, 0:ksz], wi_t[:, i, h:h + 1], sc[:, ks], ALU.mult, ALU.add,
                                      r=[r_, wi_t, sc], w=[sc])
                        yield
                for d in T_:
                    sc, st, nk = d["sc"], d["st"], d["nk"]
                    c.op("dve", lambda e: e.tensor_reduce(out=st[:, 0:1], in_=sc[:, 0:nk], axis=AX.X, op=ALU.min), r=[sc], w=[st])
                for d in T_:
                    sc, st, nk = d["sc"], d["st"], d["nk"]
                    c.op("dve", lambda e: e.tensor_reduce(out=st[:, 1:2], in_=sc[:, 0:nk], axis=AX.X, op=ALU.max), r=[sc], w=[st])
                for d in T_:
                    c.tt("dve", d["sc"][:, d["qs"]], d["sc"][:, d["qs"]], cmask[:], ALU.add, r=[d["sc"], cmask], w=[d["sc"]])
                for d in T_:
                    st = d["st"]
                    c.tt("dve", st[:, 1:2], st[:, 1:2], st[:, 0:1], ALU.subtract, r=[st], w=[st])
                for d in T_:
                    c.ts("dve", d["wk"][:], pw2[:], d["st"][:, 1:2], None, ALU.mult, None, r=[pw2, d["st"]], w=[d["wk"]])
                yield
                for it in range(NIT):
                    for d in T_:
                        st, wk = d["st"], d["wk"]
                        c.tt("dve", st[:, 2:3], st[:, 0:1], wk[:, it:it + 1], ALU.add, r=[st, wk], w=[st])
                    for d in T_:
                        st, sc, nk = d["st"], d["sc"], d["nk"]
                        c.ts("dve", d["junk"][:, 0:nk], sc[:, 0:nk], st[:, 2:3], 0.0, ALU.is_ge, ALU.add, r=[sc, st], w=[d["junk"], st],
                             accum_out=st[:, 3:4])
                    for d in T_:
                        st, wk = d["st"], d["wk"]
                        c.ts("dve", st[:, 4:5], st[:, 3:4], TOPK - 0.5, wk[:, it:it + 1], ALU.is_ge, ALU.mult, r=[st, wk], w=[st])
                    for d in T_:
                        st = d["st"]
                        c.tt("dve", st[:, 0:1], st[:, 0:1], st[:, 4:5], ALU.add, r=[st], w=[st])
                    yield
                for d in T_:
                    c.ts("dve", d["maskb"][:, 0:d["nk"]], d["sc"][:, 0:d["nk"]], d["st"][:, 0:1], None, ALU.is_ge, None,
                         r=[d["sc"], d["st"]], w=[d["maskb"]])
                for d in T_:
                    i, maskb, mT_sb = d["i"], d["maskb"], d["mT"]
                    for j0 in range(0, i + 1, 8):
                        nj = min(8, i + 1 - j0)
                        pb = PSB[(j0 // 8) % 2]
                        c.trg([(pb.t[:, (j - j0) * 128:(j - j0 + 1) * 128], maskb[:, j * 128:(j + 1) * 128]) for j in range(j0, j0 + nj)],
                              ident[:], r=[maskb, ident], w=[pb])
                        c.copy("act", mT_sb[:, j0:j0 + nj, :], pb.t[:, 0:nj * 128].rearrange("p (a b) -> p a b", b=128),
                               r=[pb], w=[mT_sb])
                        yield

            def att_steps(i):
                qs = slice(i * 128, (i + 1) * 128)
                mT_sb, q_t = mTd[i % 4], qT[i % 4]
                for g in range(2):
                    po, pd = PS[4], PS[5]
                    qg = q_t[:, 4 * g:4 * g + 4, :].rearrange("p a b -> p (a b)")

                    def st_mm(j):
                        pS = PS[2 + j % 2]
                        c.mm(pS[:], kT[:, g, j * 128:(j + 1) * 128], qg, True, True, r=[kT, q_t], w=[pS])

                    EP = {}

                    def ex(j):
                        E_, P_ = Eb[cntE["n"] % 3], Pb[cntE["n"] % 3]
                        cntE["n"] += 1
                        EP[j] = (E_, P_)
                        pS = PS[2 + j % 2]
                        c.act(E_[:], pS[:], AF.Exp, r=[pS], w=[E_], scale=scale_att)

                    st_mm(0)
                    ex(0)
                    if i >= 1:
                        st_mm(1)
                    for j in range(i + 1):
                        E_, P_ = EP.pop(j)
                        c.tt("pool", P_[:].rearrange("p (a b) -> p a b", b=128),
                             E_[:].rearrange("p (a b) -> p a b", b=128),
                             mT_sb[:, j:j + 1, :].to_broadcast([128, 4, 128]), ALU.mult, r=[E_, mT_sb], w=[P_])
                        if j + 1 <= i:
                            ex(j + 1)
                        if j + 2 <= i:
                            st_mm(j + 2)
                        c.mmg([(po[:], vt[:, j, g * 128:(g + 1) * 128], P_[:], j == 0, j == i),
                               (pd[:], ones[:], P_[:], j == 0, j == i)], r=[vt, ones, P_], w=[po, pd])
                        yield
                    fo, fd = fin_o[cntE["f"] % 2], fin_d[cntE["f"] % 2]
                    cntE["f"] += 1
                    c.copy("act", fd[:], pd[:], r=[pd], w=[fd])
                    c.copy("act", fo[:], po[:], r=[po], w=[fo])
                    c.op("dve", lambda e: e.reciprocal(out=fd[:], in_=fd[:]), r=[fd], w=[fd])
                    ya = yat[g]
                    c.tt("dve", ya[:], fo[:], fd[:], ALU.mult, r=[fo, fd], w=[ya])
                    c.dma("sp", lambda e: e.dma_start(out=s_yatt[4 * g:4 * g + 4, :, qs].rearrange("h p t -> p h t"),
                                                      in_=ya[:].rearrange("p (a b) -> p a b", b=128)), r=[ya])
                    yield

            def n_idx(tiles):
                nkb = (128 * (max(tiles) + 1) + 511) // 512
                return nkb * 8 + 1 + NIT + sum((i + 8) // 8 for i in tiles)

            def att_pair(tiles):
                for i in tiles:
                    for _ in att_steps(i):
                        yield

            def drain(g):
                for _ in g:
                    pass

            pairs = [list(range(p, min(p + 2, NT))) for p in range(0, NT, 2)]
            drain(idx_steps(pairs[0]))
            for pi, tiles in enumerate(pairs):
                ga = att_pair(tiles)
                if pi + 1 < len(pairs):
                    gi = idx_steps(pairs[pi + 1])
                    na, ni = sum(2 * (i + 2) for i in tiles), n_idx(pairs[pi + 1])
                    da = di = 0
                    fa = fi = False
                    while not (fa and fi):
                        if not fi and (fa or di * na <= da * ni):
                            try:
                                next(gi)
                                di += 1
                            except StopIteration:
                                fi = True
                        else:
                            try:
                                next(ga)
                                da += 1
                            except StopIteration:
                                fa = True
                else:
                    drain(ga)
            c.barrier()

        if PHASES >= 4:
          with ExitStack() as es:
            wout = sb(es, "wout", [128, 8, D], BF16)
            for kc in range(8):
                load_bf16(wout, wout[:, kc, :], w_out[kc * 128:(kc + 1) * 128, :])
            mqT = [sb(es, "mqT%d" % i, [128, 8, BLK], BF16) for i in range(2)]
            mrg = [sb(es, "mrg%d" % i, [128, 8, BLK], BF16) for i in range(2)]
            Em = [sb(es, "Em%d" % i, [128, BLK], BF16) for i in range(4)]
            gl = [sb(es, "gl%d" % i, [128, 5, BLK], BF16) for i in range(4)]
            ym = [sb(es, "ym%d" % i, [128, BLK], F32) for i in range(4)]
            m1 = [sb(es, "m1_%d" % i, [128, BLK], F32) for i in range(4)]
            m2 = [sb(es, "m2_%d" % i, [128, BLK], F32) for i in range(4)]
            rdm = [sb(es, "rdm%d" % i, [128, BLK], F32) for i in range(2)]
            xt = [sb(es, "xt%d" % i, [128, D], F32) for i in range(2)]
            x1t = [sb(es, "x1t%d" % i, [128, D], F32) for i in range(2)]
            scale_mem = 256.0 ** -0.5
            ncnt = 0
            for blk in range(NB):
                sl = slice(blk * BLK, (blk + 1) * BLK)
                mq_ = mqT[blk % 2]
                mg_ = mrg[blk % 2]
                c.dma("sp", lambda e: e.dma_start(out=mq_[:], in_=s_mq[:, :, sl].rearrange("h p t -> p h t")), w=[mq_])
                for hm in range(4):
                    for mt in range(2):
                        pS = PS[mt]
                        for cc in range(2):
                            c.mm(pS[:, 0:BLK], mkT[:, 2 * hm + cc, mt * 128:(mt + 1) * 128], mq_[:, 2 * hm + cc, :], cc == 0, cc == 1,
                                 r=[mkT, mq_], w=[pS])
                        c.act(Em[(hm % 2) * 2 + mt][:], pS[:, 0:BLK], AF.Exp, r=[pS], w=[Em[(hm % 2) * 2 + mt]], scale=scale_mem)
                    pd = PS[4]
                    for mt in range(2):
                        c.mm(pd[:, 0:BLK], ones[:], Em[(hm % 2) * 2 + mt][:], mt == 0, mt == 1, r=[ones, Em[(hm % 2) * 2 + mt]], w=[pd])
                    rd = rdm[hm % 2]
                    c.op("dve", lambda e: e.reciprocal(out=rd[:], in_=pd[:, 0:BLK]), r=[pd], w=[rd])
                    for cc in range(2):
                        ch = 2 * hm + cc
                        po = PS[2 + cc]
                        for mt in range(2):
                            c.mm(po[:, 0:BLK], mv[:, mt, hm * 256 + cc * 128:hm * 256 + (cc + 1) * 128], Em[(hm % 2) * 2 + mt][:],
                                 mt == 0, mt == 1, r=[mv, Em[(hm % 2) * 2 + mt]], w=[po])
                        g_ = gl[ncnt % 4]
                        y_, a_, b_ = ym[ncnt % 4], m1[ncnt % 4], m2[ncnt % 4]
                        ncnt += 1
                        for k_, src in enumerate((s_g[ch], s_g[8 + ch], s_g[16 + ch], s_ylru[ch], s_yatt[ch])):
                            c.dma("sp", lambda e: e.dma_start(out=g_[:, k_, :], in_=src[:, sl]), w=[g_])
                        c.tt("dve", y_[:], po[:, 0:BLK], rd[:], ALU.mult, r=[po, rd], w=[y_])
                        c.tt("pool", y_[:], y_[:], g_[:, 2, :], ALU.mult, r=[y_, g_], w=[y_])
                        c.tt("pool", a_[:], g_[:, 0, :], g_[:, 3, :], ALU.mult, r=[g_], w=[a_])
                        c.tt("dve", b_[:], g_[:, 1, :], g_[:, 4, :], ALU.mult, r=[g_], w=[b_])
                        c.tt("pool", a_[:], a_[:], b_[:], ALU.add, r=[a_, b_], w=[a_])
                        c.tt("dve", mg_[:, ch, :], a_[:], y_[:], ALU.add, r=[a_, y_], w=[mg_])
                for tt_ in range(BLK // 128):
                    t = blk * (BLK // 128) + tt_
                    xx, x1_ = xt[t % 2], x1t[t % 2]
                    c.dma("sp", lambda e: e.dma_start(out=xx[:], in_=x[t * 128:(t + 1) * 128, :]), w=[xx])
                    for half in range(2):
                        pO = PS[half]
                        c.mmg([(pO[:], mg_[:, ch, tt_ * 128:(tt_ + 1) * 128], wout[:, ch, half * 512:(half + 1) * 512], ch == 0, ch == 7)
                               for ch in range(8)], r=[mg_, wout], w=[pO])
                        c.tt("dve", x1_[:, half * 512:(half + 1) * 512], pO[:], xx[:, half * 512:(half + 1) * 512], ALU.add,
                             r=[pO, xx], w=[x1_])
                    c.dma("sp", lambda e: e.dma_start(out=s_x1[t * 128:(t + 1) * 128, :], in_=x1_[:]), r=[x1_])
            c.barrier()

        if PHASES >= 5:
          with ExitStack() as es:
            wq_t = sb(es, "wq_t", [128, 8, 2048], BF16)
            for kc in range(8):
                load_bf16(wq_t, wq_t[:, kc, :], w_q[kc * 128:(kc + 1) * 128, :])
            sk_t = sb(es, "sk_t", [128, 16, 128], BF16)
            load_bf16(sk_t, sk_t[:].rearrange("p a b -> p (a b)"), skT)
            gff = sb(es, "gff", [128, D], F32)
            c.dma("sp", lambda e: e.dma_start(out=gff[:], in_=nffn.partition_broadcast(128)), w=[gff])
            iota16 = sb(es, "iota16", [128, 16], F32)
            for k_ in range(16):
                c.op("pool", lambda e: e.memset(iota16[:, k_:k_ + 1], float(k_)), w=[iota16])
            NUV, NDG = 20, 4
            UVr = [sb(es, "UVr%d" % i, [128, 2 * D], BF16) for i in range(NUV)]
            dg = [sb(es, "dg%d" % i, [128, 128], BF16) for i in range(NDG)]
            junkU = sb(es, "junkU", [128, D], BF16)
            x1r = [sb(es, "x1r%d" % i, [128, D], F32) for i in range(2)]
            qtm = sb(es, "qtm", [128, 2048], BF16)
            jk5 = qtm
            r5 = [sb(es, "r5_%d" % i, [128, 1], F32) for i in range(2)]
            xnb = [sb(es, "xnb%d" % i, [128, D], BF16) for i in range(2)]
            xnT = [sb(es, "xnT%d" % i, [128, 8, 128], BF16) for i in range(2)]
            qTb = [sb(es, "qTb%d" % i, [128, 16, 128], BF16) for i in range(1)] * 2
            scs = [sb(es, "scs%d" % i, [128, 16, 128], F32) for i in range(1)] * 2
            scw = [sb(es, "scw%d" % i, [128, 128], F32) for i in range(2)]
            s16 = [sb(es, "s16_%d" % i, [128, 16, 16], F32) for i in range(2)]
            i16 = [sb(es, "i16_%d" % i, [128, 16, 16], U32) for i in range(2)]
            i16f = [sb(es, "i16f%d" % i, [128, 16, 16], F32) for i in range(2)]
            cand = [sb(es, "cand%d" % i, [128, 8, 256], F32) for i in range(1)] * 2
            cwk = [sb(es, "cwk%d" % i, [128, 256], F32) for i in range(2)]
            tops = [sb(es, "tops%d" % i, [128, 8, 16], F32) for i in range(2)]
            posu = [sb(es, "posu%d" % i, [128, 8, 16], U32) for i in range(2)]
            pab = [sb(es, "pab%d" % i, [128, 2, 128], U32) for i in range(2)]
            pabf = [sb(es, "pabf%d" % i, [128, 2, 128], F32) for i in range(2)]
            eq = [sb(es, "eq%d" % i, [128, 8, 16, 16], F32) for i in range(1)] * 2
            isel = [sb(es, "isel%d" % i, [128, 2, 128], F32) for i in range(2)]
            eidf = [sb(es, "eidf%d" % i, [128, 128], F32) for i in range(2)]
            eidx = [sb(es, "eidx%d" % i, [128, 128], I32) for i in range(2)]
            gsm = [sb(es, "gsm%d" % i, [128, 8, 16], F32) for i in range(2)]
            ssum = [sb(es, "ssum%d" % i, [128, 8], F32) for i in range(2)]
            scr = [sb(es, "scr%d" % i, [128, 128], F32) for i in range(2)]
            ga = [sb(es, "ga%d" % i, [128, 16], F32) for i in range(2)]
            gb = [sb(es, "gb%d" % i, [128, 16], F32) for i in range(2)]
            actw = [sb(es, "actw%d" % i, [128, 16], F32) for i in range(2)]
            yo = [sb(es, "yo%d" % i, [128, D], F32) for i in range(1)] * 2
            cnt5 = {"uv": 0, "d": 0, "h": 0}

            def peer_select(t):
                b = t % 2
                x1_ = x1r[b]
                c.dma("sp", lambda e: e.dma_start(out=x1_[:], in_=s_x1[t * 128:(t + 1) * 128, :]), w=[x1_])
                yield
                c.act(jk5[:, 0:D], x1_[:], AF.Square, r=[x1_], w=[jk5, r5[b]], accum_out=r5[b][:, 0:1])
                c.act(r5[b][:], r5[b][:], AF.Sqrt, r=[r5[b], epsb], w=[r5[b]], bias=epsb[:, 0:1], scale=1.0 / D)
                yield
                c.op("dve", lambda e: e.reciprocal(out=r5[b][:], in_=r5[b][:]), r=[r5[b]], w=[r5[b]])
                c.stt("dve", xnb[b][:], x1_[:], r5[b][:, 0:1], gff[:], ALU.mult, ALU.mult, r=[x1_, r5[b], gff], w=[xnb[b]])
                yield
                pb = PSB[t % 2]
                c.trg([(pb.t[:, kc * 128:(kc + 1) * 128], xnb[b][:, kc * 128:(kc + 1) * 128]) for kc in range(8)], ident[:],
                      r=[xnb[b], ident], w=[pb])
                yield
                c.copy("act", xnT[b][:], pb.t[:].rearrange("p (a b) -> p a b", b=128), r=[pb], w=[xnT[b]])
                yield
                for cg in range(5):
                    if cg < 4:
                        ps = PS[cg % 2]
                        c.mmg([(ps[:], xnT[b][:, kc, :], wq_t[:, kc, cg * 512:(cg + 1) * 512], kc == 0, kc == 7) for kc in range(8)],
                              r=[xnT[b], wq_t], w=[ps])
                    if cg >= 1:
                        ps = PS[(cg - 1) % 2]
                        c.copy("act", qtm[:, (cg - 1) * 512:cg * 512], ps[:], r=[ps], w=[qtm])
                    yield
                for g8 in range(3):
                    if g8 < 2:
                        pbq = PSB[(t + 1 + g8) % 2]
                        c.trg([(pbq.t[:, j * 128:(j + 1) * 128], qtm[:, (g8 * 8 + j) * 128:(g8 * 8 + j + 1) * 128]) for j in range(8)],
                              ident[:], r=[qtm, ident], w=[pbq])
                    if g8 >= 1:
                        pbq = PSB[(t + g8) % 2]
                        c.copy("act", qTb[b][:, (g8 - 1) * 8:g8 * 8, :], pbq.t[:].rearrange("p (a b) -> p a b", b=128), r=[pbq], w=[qTb[b]])
                    yield
                for g4 in range(5):
                    if g4 < 4:
                        ps = PS[2 + g4 % 2]
                        c.mmg([(ps[:, j * 128:(j + 1) * 128], qTb[b][:, g4 * 4 + j, :], sk_t[:, g4 * 4 + j, :], True, True) for j in range(4)],
                              r=[qTb[b], sk_t], w=[ps])
                    if g4 >= 1:
                        ps = PS[2 + (g4 - 1) % 2]
                        c.copy("act", scs[b][:, (g4 - 1) * 4:g4 * 4, :], ps[:].rearrange("p (a b) -> p a b", b=128), r=[ps], w=[scs[b]])
                    yield
                for hp in range(16):
                    w_ = scw[hp % 2]
                    c.op("dve", lambda e: e.max(out=s16[b][:, hp, 0:8], in_=scs[b][:, hp, :]), r=[scs[b]], w=[s16[b]])
                    c.op("dve", lambda e: e.match_replace(out=w_[:], in_to_replace=s16[b][:, hp, 0:8], in_values=scs[b][:, hp, :],
                                                          imm_value=NEG), r=[s16[b], scs[b]], w=[w_])
                    c.op("dve", lambda e: e.max(out=s16[b][:, hp, 8:16], in_=w_[:]), r=[w_], w=[s16[b]])
                    c.op("dve", lambda e: e.max_index(out=i16[b][:, hp, 0:8], in_max=s16[b][:, hp, 0:8], in_values=scs[b][:, hp, :]),
                         r=[s16[b], scs[b]], w=[i16[b]])
                    c.op("dve", lambda e: e.max_index(out=i16[b][:, hp, 8:16], in_max=s16[b][:, hp, 8:16], in_values=scs[b][:, hp, :]),
                         r=[s16[b], scs[b]], w=[i16[b]])
                    yield
                c.copy("dve", i16f[b][:], i16[b][:], r=[i16[b]], w=[i16f[b]])
                s16v = s16[b][:].rearrange("p (h two) k -> p h two k", two=2)
                yield
                yield
                c.tt("pool", cand[b][:].rearrange("p h (a b) -> p h a b", b=16),
                     s16v[:, :, 0, :].unsqueeze(3).to_broadcast([128, 8, 16, 16]),
                     s16v[:, :, 1, :].unsqueeze(2).to_broadcast([128, 8, 16, 16]), ALU.add, r=[s16[b]], w=[cand[b]])
                yield
                yield
                yield
                for h in range(8):
                    w_ = cwk[h % 2]
                    c.op("dve", lambda e: e.max(out=tops[b][:, h, 0:8], in_=cand[b][:, h, :]), r=[cand[b]], w=[tops[b]])
                    c.op("dve", lambda e: e.match_replace(out=w_[:], in_to_replace=tops[b][:, h, 0:8], in_values=cand[b][:, h, :],
                                                          imm_value=NEG), r=[tops[b], cand[b]], w=[w_])
                    c.op("dve", lambda e: e.max(out=tops[b][:, h, 8:16], in_=w_[:]), r=[w_], w=[tops[b]])
                    c.op("dve", lambda e: e.max_index(out=posu[b][:, h, 0:8], in_max=tops[b][:, h, 0:8], in_values=cand[b][:, h, :]),
                         r=[tops[b], cand[b]], w=[posu[b]])
                    c.op("dve", lambda e: e.max_index(out=posu[b][:, h, 8:16], in_max=tops[b][:, h, 8:16], in_values=cand[b][:, h, :]),
                         r=[tops[b], cand[b]], w=[posu[b]])
                    yield
                pflat = posu[b][:].rearrange("p h k -> p (h k)")
                c.op("dve", lambda e: e.tensor_single_scalar(out=pab[b][:, 0, :], in_=pflat, scalar=4, op=ALU.logical_shift_right),
                     r=[posu[b]], w=[pab[b]])
                c.op("dve", lambda e: e.tensor_single_scalar(out=pab[b][:, 1, :], in_=pflat, scalar=15, op=ALU.bitwise_and),
                     r=[posu[b]], w=[pab[b]])
                c.copy("dve", pabf[b][:], pab[b][:], r=[pab[b]], w=[pabf[b]])
                i16v = i16f[b][:].rearrange("p (h two) k -> p h two k", two=2)
                for side in range(2):
                    sel = pabf[b][:, side, :].rearrange("p (h k) -> p h k", k=16)
                    c.tt("dve", eq[b][:], sel.unsqueeze(3).to_broadcast([128, 8, 16, 16]),
                         iota16[:].unsqueeze(1).unsqueeze(1).to_broadcast([128, 8, 16, 16]), ALU.is_equal,
                         r=[pabf[b], iota16], w=[eq[b]])
                    yield
                    yield
                    c.tt("pool", eq[b][:], eq[b][:], i16v[:, :, side, :].unsqueeze(2).to_broadcast([128, 8, 16, 16]), ALU.mult,
                         r=[eq[b], i16f[b]], w=[eq[b]])
                    yield
                    yield
                    yield
                    c.op("dve", lambda e: e.tensor_reduce(out=isel[b][:, side, :], in_=eq[b][:].rearrange("p h k a -> p (h k) a"),
                                                          axis=AX.X, op=ALU.add), r=[eq[b]], w=[isel[b]])
                    yield
                c.stt("dve", eidf[b][:], isel[b][:, 0, :], 128.0, isel[b][:, 1, :], ALU.mult, ALU.add, r=[isel[b]], w=[eidf[b]])
                c.copy("dve", eidx[b][:], eidf[b][:], r=[eidf[b]], w=[eidx[b]])
                c.tt("dve", gsm[b][:], tops[b][:], tops[b][:, :, 0:1].to_broadcast([128, 8, 16]), ALU.subtract, r=[tops[b]], w=[gsm[b]])
                c.act(gsm[b][:], gsm[b][:], AF.Exp, r=[gsm[b]], w=[gsm[b]])
                c.op("dve", lambda e: e.tensor_reduce(out=ssum[b][:], in_=gsm[b][:], axis=AX.X, op=ALU.add), r=[gsm[b]], w=[ssum[b]])
                c.op("dve", lambda e: e.reciprocal(out=ssum[b][:], in_=ssum[b][:]), r=[ssum[b]], w=[ssum[b]])
                c.tt("dve", gsm[b][:], gsm[b][:], ssum[b][:].unsqueeze(2).to_broadcast([128, 8, 16]), ALU.mult, r=[gsm[b], ssum[b]], w=[gsm[b]])

            def peer_compute(t, selgen):
                b = t % 2
                x1_ = x1r[b]
                acc = (PS[4], PS[5])

                def dots(hd, k0, k1, vts):
                    for k_ in range(k0, k1):
                        slot = hd * 16 + k_
                        UV_ = UVr[cnt5["uv"] % NUV]
                        cnt5["uv"] += 1
                        vts.append(UV_)
                        c.dma("pool", lambda e: e.indirect_dma_start(out=UV_[:], out_offset=None, in_=s_uvb,
                                                                     in_offset=bass.IndirectOffsetOnAxis(ap=eidx[b][:, slot:slot + 1], axis=0)),
                              r=[eidx[b]], w=[UV_])
                        c.stt("dve", junkU[:], UV_[:, 0:D], 1.0, xnb[b][:], ALU.mult, ALU.mult, r=[UV_, xnb[b]], w=[junkU, scr[b]],
                              accum_out=scr[b][:, slot:slot + 1])
                        if selgen is not None and ((slot < 36 and slot % 2 == 0) or (slot >= 40 and slot % 2 == 0)):
                            next(selgen, None)

                BS = 8
                NBT = 128 // BS
                vnext = []
                dots(0, 0, BS, vnext)
                for bt in range(NBT):
                    vts = vnext
                    vnext = []
                    hb_ = cnt5["h"] % 2
                    cnt5["h"] += 1
                    s0 = bt * BS
                    sx = scr[b][:, s0:s0 + BS]
                    gsl = gsm[b][:].rearrange("p h k -> p (h k)")[:, s0:s0 + BS]
                    nhd, nk0 = (s0 + BS) // 16, (s0 + BS) % 16
                    if bt + 1 < NBT:
                        dots(nhd, nk0, nk0 + 2, vnext)
                    c.stt("dve", ga[hb_][:, 0:BS], sx, 0.044715, sx, ALU.mult, ALU.mult, r=[scr[b]], w=[ga[hb_]])
                    c.stt("dve", ga[hb_][:, 0:BS], ga[hb_][:, 0:BS], 1.0, sx, ALU.add, ALU.mult, r=[ga[hb_], scr[b]], w=[ga[hb_]])
                    c.act(gb[hb_][:, 0:BS], ga[hb_][:, 0:BS], AF.Sigmoid, r=[ga[hb_]], w=[gb[hb_]], scale=1.5957691216057308)
                    c.tt("dve", actw[hb_][:, 0:BS], sx, gsl, ALU.mult, r=[scr[b], gsm[b]], w=[actw[hb_]])
                    if bt + 1 < NBT:
                        dots(nhd, nk0 + 2, nk0 + 4, vnext)
                    c.tt("dve", actw[hb_][:, 0:BS], actw[hb_][:, 0:BS], gb[hb_][:, 0:BS], ALU.mult, r=[gb[hb_], actw[hb_]], w=[actw[hb_]])
                    for k_ in range(BS):
                        slot = s0 + k_
                        d_ = dg[cnt5["d"] % NDG]
                        cnt5["d"] += 1
                        c.act(d_[:], identf[:], AF.Copy, r=[identf, actw[hb_]], w=[d_], scale=actw[hb_][:, k_:k_ + 1])
                        c.mmg([(acc[half][:], d_[:], vts[k_][:, D + half * 512:D + (half + 1) * 512], slot == 0, slot == 127)
                               for half in range(2)], r=[d_, vts[k_]], w=[acc[0], acc[1]])
                    if bt + 1 < NBT:
                        dots(nhd, nk0 + 4, nk0 + BS, vnext)
                if selgen is not None:
                    for _ in selgen:
                        pass
                for half in range(2):
                    c.tt("dve", yo[b][:, half * 512:(half + 1) * 512], acc[half][:], x1_[:, half * 512:(half + 1) * 512], ALU.add,
                         r=[acc[half], x1_], w=[yo[b]])
                c.dma("sp", lambda e: e.dma_start(out=y[t * 128:(t + 1) * 128, :], in_=yo[b][:]), r=[yo[b]])

            for _ in peer_select(0):
                pass
            for t in range(NT):
                peer_compute(t, peer_select(t + 1) if t + 1 < NT else None)
            c.barrier()

        c.barrier(engines=("sp",))
    return nc


build.phases = 99
build.skip = set()


def rope_tables(S):
    pos = np.arange(S, dtype=np.float32)
    out = np.zeros((4, 128, S), np.float32)
    for ti, hd in ((0, 128), (2, 64)):
        half = hd // 2
        freq = (np.float32(10000.0) ** (-np.arange(half, dtype=np.float32) / np.float32(half))).astype(np.float32)
        ang = pos[None, :] * freq[:, None]
        cs, sn = np.cos(ang).astype(np.float32), np.sin(ang).astype(np.float32)
        for p in range(128):
            d = p % hd
            j = d % half
            out[ti, p] = cs[j]
            out[ti + 1, p] = -sn[j] if d < half else sn[j]
    return out.astype(ml_dtypes.bfloat16)


def _swap_cols(w, hd):
    n = w.shape[1] // hd
    w = w.reshape(w.shape[0], n, 2, hd // 2)
    return np.ascontiguousarray(w[:, :, ::-1, :]).reshape(w.shape[0], n * hd)


def host_layout(inp, S):
    w_in = np.asarray(inp["w_in"][0], np.float32)
    sp = np.cumsum([0, 1024, 1024, 1024, 256, 256, 512, 64, 8, 1024, 3072])
    lx, lg, q, k, v, qi, ki, wi, mq, gates = [w_in[:, sp[i]:sp[i + 1]] for i in range(10)]
    ki2 = np.concatenate([ki, ki], axis=1)
    w_fm = np.concatenate([lx, lg, q, _swap_cols(q, 128), k, _swap_cols(k, 128), qi, _swap_cols(qi, 64),
                           ki2, _swap_cols(ki2, 64), mq, gates], axis=1)
    assert w_fm.shape[1] == NCH * 128
    w_tm = np.concatenate([v, wi], axis=1)

    def fm(vec):
        return np.asarray(vec, np.float32).reshape(-1, 128).T

    def sw(vec, hd):
        vec = np.asarray(vec, np.float32)
        return np.concatenate([vec[hd // 2:], vec[:hd // 2]])

    ikn = np.asarray(inp["idx_k_norm"][0], np.float32)
    cols = [fm(inp["norm_mix"][0])]
    cols += [fm(inp["conv_w"][0][j]) for j in range(4)]
    cols += [fm(inp["conv_b"][0]), fm(inp["lru_ba"][0].reshape(-1)), fm(inp["lru_bi"][0].reshape(-1)), fm(inp["lru_lambda"][0])]
    cols += [fm(inp["q_norm"][0]), fm(sw(inp["q_norm"][0], 128)), fm(inp["k_norm"][0]), fm(sw(inp["k_norm"][0], 128))]
    cols += [fm(np.concatenate([ikn, ikn])), fm(np.concatenate([sw(ikn, 64), sw(ikn, 64)]))]
    cols += [fm(inp["mem_norm"][0]), fm(inp["mem_q_norm"][0]), fm(inp["mem_k_norm"][0]), fm(inp["norm_ffn"][0])]
    pvec = np.ascontiguousarray(np.concatenate(cols, axis=1))
    assert pvec.shape == (128, NPV), pvec.shape
    wbd = np.zeros((128, 8, 2, 128), np.float32)
    for ci in range(8):
        for j, nm in enumerate(("lru_wa", "lru_wi")):
            w = np.asarray(inp[nm][0], np.float32)
            wbd[0:64, ci, j, 0:64] = w[2 * ci]
            wbd[64:128, ci, j, 64:128] = w[2 * ci + 1]
    skT = np.ascontiguousarray(np.asarray(inp["peer_subkeys"][0], np.float32).reshape(16, 128, 128).transpose(2, 0, 1))
    shared = {
        "w_fm": np.ascontiguousarray(w_fm), "w_tm": np.ascontiguousarray(w_tm), "pvec": pvec,
        "wbd": wbd.reshape(128, -1), "ropet": rope_tables(S),
        "w_mem_kv": np.ascontiguousarray(inp["w_mem_kv"][0], np.float32),
        "w_out": np.ascontiguousarray(inp["w_out"][0], np.float32),
        "nffn": np.ascontiguousarray(inp["norm_ffn"][0], np.float32).reshape(1, D),
        "nmix": np.ascontiguousarray(inp["norm_mix"][0], np.float32).reshape(1, D),
        "w_q": np.ascontiguousarray(inp["peer_wq"][0], np.float32),
        "skT": skT.reshape(128, -1),
        "pu": np.ascontiguousarray(inp["peer_u"][0], np.float32),
        "pv": np.ascontiguousarray(inp["peer_v"][0], np.float32),
    }
    return shared


def kernel(**inputs):
    xs = np.asarray(inputs["x"], np.float32)
    mems = np.asarray(inputs["mem"], np.float32)
    B, S, _ = xs.shape
    shared = host_layout(inputs, S)
    nc = build(S)
    in_maps = []
    for b in range(B):
        m = dict(shared)
        m["x"] = np.ascontiguousarray(xs[b])
        m["mem"] = np.ascontiguousarray(mems[b])
        in_maps.append(m)
    res = run_bass_kernel_spmd(nc, in_maps, core_ids=list(range(B)))
    return np.stack([np.asarray(r["y"], np.float32) for r in res.results], axis=0)
```

```python
import numpy as np
import ml_dtypes
from contextlib import ExitStack
import concourse.bass as bass
import concourse.mybir as mybir
from concourse.bass_utils import run_bass_kernel_spmd

F32 = mybir.dt.float32
BF16 = mybir.dt.bfloat16
I32 = mybir.dt.int32
U32 = mybir.dt.uint32
ALU = mybir.AluOpType
AF = mybir.ActivationFunctionType
AX = mybir.AxisListType

D = 1024
EPS = 1e-6
NEG = -1.0e30

C_LX, C_LG, C_Q, C_QS, C_K, C_KS, C_QI, C_QIS, C_KI, C_KIS, C_MQ, C_G = 0, 8, 16, 24, 32, 34, 36, 40, 44, 45, 46, 54
NCH = 78
PV_NMIX, PV_CW, PV_CB, PV_BA, PV_BI, PV_LAM = 0, 8, 40, 48, 56, 64
PV_QN, PV_QNS, PV_KN, PV_KNS, PV_IKN, PV_IKNS = 72, 73, 74, 75, 76, 77
PV_MEMN, PV_MQN, PV_MKN, PV_NFFN = 78, 86, 88, 90
NPV = 98


class Res:
    __slots__ = ("w", "r")

    def __init__(self):
        self.w = None
        self.r = {}


class Tl:
    def __init__(self, t, excl=False):
        self.t = t
        self.res = Res()
        self.excl = excl

    def __getitem__(self, k):
        return self.t[k]


class Ctx:
    NDMA = {"sp": 16, "act": 4, "pool": 32}

    def __init__(self, nc, es):
        self.nc = nc
        self.eng = {"pe": nc.tensor, "dve": nc.vector, "act": nc.scalar, "pool": nc.gpsimd, "sp": nc.sync}
        self.sem = {k: es.enter_context(nc.semaphore("s_" + k)) for k in self.eng}
        self.cnt = {k: 0 for k in self.eng}
        self.waited = {k: {} for k in self.eng}
        self.dsem, self.dcnt, self.dnext = {}, {}, {}
        for k, n in self.NDMA.items():
            self.dsem[k] = [es.enter_context(nc.semaphore("d_%s%d" % (k, i))) for i in range(n)]
            self.dcnt[k] = [0] * n
            self.dnext[k] = 0
        self.nins = 0

    def _wait(self, e, ev):
        if ev is None:
            return
        sem, val = ev
        wd = self.waited[e]
        if wd.get(sem, 0) >= val:
            return
        self.eng[e].wait_ge(sem, val)
        wd[sem] = val

    def _deps(self, e, r, w):
        for x in r:
            self._wait(e, x.res.w)
        for x in w:
            self._wait(e, x.res.w)
            for sem, val in x.res.r.items():
                self._wait(e, (sem, val))

    def _commit(self, ev, r, w):
        for x in r:
            if x.res.r.get(ev[0], 0) < ev[1]:
                x.res.r[ev[0]] = ev[1]
        for x in w:
            x.res.w = ev
            x.res.r = {}

    def op(self, e, fn, r=(), w=()):
        w = list(w) + [x for x in r if x.excl and x not in w]
        self._deps(e, r, w)
        ins = fn(self.eng[e])
        self.cnt[e] += 1
        ins.then_inc(self.sem[e], 1)
        self._commit((self.sem[e], self.cnt[e]), r, w)
        self.nins += 1
        return ins

    def dma(self, e, fn, r=(), w=()):
        self._deps(e, r, w)
        i = self.dnext[e]
        self.dnext[e] = (i + 1) % len(self.dsem[e])
        sem = self.dsem[e][i]
        if self.dcnt[e][i]:
            self._wait(e, (sem, self.dcnt[e][i]))
        ins = fn(self.eng[e])
        self.dcnt[e][i] += 16
        ins.then_inc(sem, 16)
        self._commit((sem, self.dcnt[e][i]), r, w)
        self.nins += 1
        return ins

    def barrier(self, engines=("pe", "dve", "act", "pool", "sp")):
        for e in engines:
            for k in self.eng:
                if k != e and self.cnt[k]:
                    self._wait(e, (self.sem[k], self.cnt[k]))
            for k in self.dsem:
                for i, s in enumerate(self.dsem[k]):
                    if self.dcnt[k][i]:
                        self._wait(e, (s, self.dcnt[k][i]))

    def mm(self, out, lhsT, rhs, start, stop, r, w):
        return self.op("pe", lambda e: e.matmul(out, lhsT, rhs, start=start, stop=stop), r=r, w=w)

    def mmg(self, items, r, w):
        w = list(w) + [x for x in r if x.excl and x not in w]
        self._deps("pe", r, w)
        ins = None
        for (out, lhsT, rhs, st, sp) in items:
            ins = self.eng["pe"].matmul(out, lhsT, rhs, start=st, stop=sp)
            self.nins += 1
        self.cnt["pe"] += 1
        ins.then_inc(self.sem["pe"], 1)
        self._commit((self.sem["pe"], self.cnt["pe"]), r, w)
        return ins

    def trg(self, items, ident, r, w):
        w = list(w) + [x for x in r if x.excl and x not in w]
        self._deps("pe", r, w)
        ins = None
        for (out, in_) in items:
            ins = self.eng["pe"].transpose(out, in_, ident)
            self.nins += 1
        self.cnt["pe"] += 1
        ins.then_inc(self.sem["pe"], 1)
        self._commit((self.sem["pe"], self.cnt["pe"]), r, w)
        return ins

    def tr(self, out, in_, ident, r, w):
        return self.op("pe", lambda e: e.transpose(out, in_, ident), r=r, w=w)

    def act(self, out, in_, func, r, w, bias=None, scale=None, accum_out=None, eng="act"):
        kw = {}
        if bias is not None:
            kw["bias"] = bias
        if scale is not None:
            kw["scale"] = scale
        if accum_out is not None:
            kw["accum_out"] = accum_out
        return self.op(eng, lambda e: e.activation(out=out, in_=in_, func=func, **kw), r=r, w=w)

    def ts(self, eng, out, in0, s1, s2, op0, op1, r, w, accum_out=None):
        if op1 is None:
            return self.op(eng, lambda e: e.tensor_scalar(out=out, in0=in0, scalar1=s1, scalar2=None, op0=op0), r=r, w=w)
        kw = {}
        if accum_out is not None:
            kw["accum_out"] = accum_out
        return self.op(eng, lambda e: e.tensor_scalar(out=out, in0=in0, scalar1=s1, scalar2=s2, op0=op0, op1=op1, **kw),
                       r=r, w=w)

    def tt(self, eng, out, in0, in1, op, r, w):
        return self.op(eng, lambda e: e.tensor_tensor(out=out, in0=in0, in1=in1, op=op), r=r, w=w)

    def stt(self, eng, out, in0, scalar, in1, op0, op1, r, w, accum_out=None):
        kw = {}
        if accum_out is not None:
            kw["accum_out"] = accum_out
        return self.op(eng, lambda e: e.scalar_tensor_tensor(out=out, in0=in0, scalar=scalar, in1=in1, op0=op0, op1=op1, **kw),
                       r=r, w=w)

    def copy(self, eng, out, in_, r, w):
        if eng == "act":
            return self.op("act", lambda e: e.copy(out=out, in_=in_), r=r, w=w)
        return self.op(eng, lambda e: e.tensor_copy(out=out, in_=in_), r=r, w=w)


def build(S, dbg=False):
    NT = S // 128
    BLK = min(512, S)
    NB = S // BLK
    TOPK = min(256, S // 4)
    nc = bass.Bass("TRN2", target_bir_lowering=False)

    def din(name, shape, dt=F32):
        return nc.dram_tensor(name, shape, dt, kind="ExternalInput").ap()

    def dscr(name, shape, dt):
        return nc.dram_tensor(name, shape, dt, kind="ExternalOutput" if dbg else "Internal").ap()

    x = din("x", [S, D])
    mem = din("mem", [256, D])
    w_fm = din("w_fm", [D, NCH * 128])
    w_tm = din("w_tm", [D, 264])
    pvec = din("pvec", [128, NPV])
    wbd = din("wbd", [128, 8 * 2 * 128])
    ropet = din("ropet", [4, 128, S], BF16)
    w_mem_kv = din("w_mem_kv", [D, 2048])
    w_out = din("w_out", [D, D])
    nffn = din("nffn", [1, D])
    nmix = din("nmix", [1, D])
    w_q = din("w_q", [D, 2048])
    skT = din("skT", [128, 16 * 128])
    pu = din("pu", [16384, D])
    pv = din("pv", [16384, D])
    y = nc.dram_tensor("y", [S, D], F32, kind="ExternalOutput").ap()

    s_q = dscr("s_q", [8, 128, S], BF16)
    s_k = dscr("s_k", [2, 128, S], BF16)
    s_v = dscr("s_v", [S, 256], BF16)
    s_wi = dscr("s_wi", [S, 8], F32)
    s_qi = dscr("s_qi", [4, 128, S], BF16)
    s_ki = dscr("s_ki", [128, S], BF16)
    s_mq = dscr("s_mq", [8, 128, S], BF16)
    s_g = dscr("s_g", [24, 128, S], BF16)
    s_ylru = dscr("s_ylru", [8, 128, S], BF16)
    s_yatt = dscr("s_yatt", [8, 128, S], BF16)
    s_x1 = dscr("s_x1", [S, D], F32)
    s_uvb = nc.dram_tensor("s_uvb", [16384, 2 * D], BF16, kind="Internal").ap()

    with ExitStack() as es0:
        es0.enter_context(nc.allow_low_precision("bf16 matmul operands, fp32 accumulation"))
        es0.enter_context(nc.allow_non_contiguous_dma("small strided loads"))
        c = Ctx(nc, es0)

        def sb(es, name, shape, dt):
            return Tl(es.enter_context(nc.sbuf_tensor(name, shape, dt)))

        PS = [Tl(es0.enter_context(nc.psum_tensor("ps%d" % i, [128, 512], F32)), True) for i in range(6)]
        PSB = [Tl(es0.enter_context(nc.psum_tensor("psb%d" % i, [128, 1024], BF16)), True) for i in range(2)]

        ident = sb(es0, "ident", [128, 128], BF16)
        identf = sb(es0, "identf", [128, 128], F32)
        ones = sb(es0, "ones", [128, 128], BF16)
        onesm = sb(es0, "onesm", [128, 3 * 128], BF16)
        pv_t = sb(es0, "pvec_sb", [128, NPV], F32)
        c.op("pool", lambda e: e.memset(identf[:], 0.0), w=[identf])
        c.op("pool", lambda e: e.affine_select(out=identf[:], in_=identf[:], pattern=[[-1, 128]],
                                               compare_op=ALU.not_equal, fill=1.0, base=0, channel_multiplier=1),
             r=[identf], w=[identf])
        c.copy("dve", ident[:], identf[:], r=[identf], w=[ident])
        c.op("dve", lambda e: e.memset(ones[:], 1.0), w=[ones])
        zeros = sb(es0, "zeros", [128, 128], BF16)
        c.op("dve", lambda e: e.memset(zeros[:], 0.0), w=[zeros])
        c.op("dve", lambda e: e.memset(onesm[:, 0:128], 1.0 / 128), w=[onesm])
        c.op("dve", lambda e: e.memset(onesm[:, 128:256], 0.0), w=[onesm])
        c.op("dve", lambda e: e.memset(onesm[0:64, 128:192], 1.0 / 64), w=[onesm])
        c.op("dve", lambda e: e.memset(onesm[64:128, 192:256], 1.0 / 64), w=[onesm])
        c.op("dve", lambda e: e.memset(onesm[:, 256:384], 1.0 / 256), w=[onesm])
        c.dma("sp", lambda e: e.dma_start(out=pv_t[:], in_=pvec), w=[pv_t])
        perm = sb(es0, "perm", [128, 2, 128], BF16)
        c.copy("dve", perm[:, 0, 0:64], ident[:, 64:128], r=[ident], w=[perm])
        c.copy("dve", perm[:, 0, 64:128], ident[:, 0:64], r=[ident], w=[perm])
        for q4 in range(4):
            src = (q4 ^ 1) * 32
            c.copy("dve", perm[:, 1, q4 * 32:(q4 + 1) * 32], ident[:, src:src + 32], r=[ident], w=[perm])


        def load_bf16(dst_tl, dst_ap, src_ap):
            c.dma("pool", lambda e: e.dma_start(out=dst_ap, in_=src_ap), w=[dst_tl])

        def rstd_from_ms(out_ap, ms_ap, r, w):
            c.act(out_ap, ms_ap, AF.Ln, r=r, w=w, bias=epsb[:, 0:1], scale=1.0)
            c.act(out_ap, out_ap, AF.Exp, r=w, w=w, scale=-0.5)

        epsb = sb(es0, "epsb", [128, 2], F32)
        c.op("dve", lambda e: e.memset(epsb[:, 0:1], EPS), w=[epsb])
        c.op("dve", lambda e: e.memset(epsb[:, 1:2], 1.0), w=[epsb])

        def rmsnorm_rows(es, name, src_tl, width):
            junk = sb(es, name + "_junk", [128, width], BF16)
            ss = sb(es, name + "_ss", [128, 1], F32)
            c.act(junk[:], src_tl[:, 0:width], AF.Square, r=[src_tl], w=[junk, ss], accum_out=ss[:, 0:1])
            c.act(ss[:], ss[:], AF.Sqrt, r=[ss, epsb], w=[ss], bias=epsb[:, 0:1], scale=1.0 / width)
            c.op("dve", lambda e: e.reciprocal(out=ss[:], in_=ss[:]), r=[ss], w=[ss])
            return ss

        mkT = sb(es0, "mkT", [128, 8, 256], BF16)
        mv = sb(es0, "mv", [128, 2, D], BF16)
        with ExitStack() as es:
            wkv = sb(es, "wkv", [128, 8, 2048], BF16)
            for kc in range(8):
                load_bf16(wkv, wkv[:, kc, :], w_mem_kv[kc * 128:(kc + 1) * 128, :])
            mT = sb(es, "mT", [128, 8, 256], BF16)
            for mt in range(2):
                mx = sb(es, "mx%d" % mt, [128, D], F32)
                c.dma("sp", lambda e: e.dma_start(out=mx[:], in_=mem[mt * 128:(mt + 1) * 128, :]), w=[mx])
                rs = rmsnorm_rows(es, "mrs%d" % mt, mx, D)
                mh = sb(es, "mh%d" % mt, [128, D], BF16)
                c.act(mh[:], mx[:], AF.Copy, r=[mx, rs], w=[mh], scale=rs[:, 0:1])
                for kc in range(8):
                    pb = PSB[kc % 2]
                    po = pb.t[:, 0:128]
                    c.tr(po, mh[:, kc * 128:(kc + 1) * 128], ident[:], r=[mh, ident], w=[pb])
                    c.ts("dve", mT[:, kc, mt * 128:(mt + 1) * 128], po, pv_t[:, PV_MEMN + kc:PV_MEMN + kc + 1], None,
                         ALU.mult, None, r=[pb, pv_t], w=[mT])
            for mt in range(2):
                mkf = sb(es, "mkf%d" % mt, [128, D], F32)
                for half in range(2):
                    ps = PS[half]
                    for kc in range(8):
                        c.mm(ps[:], mT[:, kc, mt * 128:(mt + 1) * 128], wkv[:, kc, half * 512:(half + 1) * 512],
                             kc == 0, kc == 7, r=[mT, wkv], w=[ps])
                    c.copy("act", mkf[:, half * 512:(half + 1) * 512], ps[:], r=[ps], w=[mkf])
                mkb = sb(es, "mkb%d" % mt, [128, D], BF16)
                for h in range(4):
                    junk = sb(es, "mkj%d_%d" % (mt, h), [128, 256], BF16)
                    ss = sb(es, "mks%d_%d" % (mt, h), [128, 1], F32)
                    c.act(junk[:], mkf[:, h * 256:(h + 1) * 256], AF.Square, r=[mkf], w=[junk, ss], accum_out=ss[:, 0:1])
                    c.act(ss[:], ss[:], AF.Sqrt, r=[ss, epsb], w=[ss], bias=epsb[:, 0:1], scale=1.0 / 256)
                    c.op("dve", lambda e: e.reciprocal(out=ss[:], in_=ss[:]), r=[ss], w=[ss])
                    c.act(mkb[:, h * 256:(h + 1) * 256], mkf[:, h * 256:(h + 1) * 256], AF.Copy, r=[mkf, ss], w=[mkb],
                          scale=ss[:, 0:1])
                for kc in range(8):
                    pb = PSB[kc % 2]
                    po = pb.t[:, 0:128]
                    c.tr(po, mkb[:, kc * 128:(kc + 1) * 128], ident[:], r=[mkb, ident], w=[pb])
                    gcol = PV_MKN + (kc % 2)
                    c.ts("dve", mkT[:, kc, mt * 128:(mt + 1) * 128], po, pv_t[:, gcol:gcol + 1], None,
                         ALU.mult, None, r=[pb, pv_t], w=[mkT])
                for half in range(2):
                    ps = PS[2 + half]
                    for kc in range(8):
                        c.mm(ps[:], mT[:, kc, mt * 128:(mt + 1) * 128], wkv[:, kc, 1024 + half * 512:1024 + (half + 1) * 512],
                             kc == 0, kc == 7, r=[mT, wkv], w=[ps])
                    c.copy("act", mv[:, mt, half * 512:(half + 1) * 512], ps[:], r=[ps], w=[mv])
            c.barrier()

        if dbg:
            d_mkT = nc.dram_tensor("d_mkT", [128, 8 * 256], BF16, kind="ExternalOutput").ap()
            d_mv = nc.dram_tensor("d_mv", [128, 2 * D], BF16, kind="ExternalOutput").ap()
            c.dma("sp", lambda e: e.dma_start(out=d_mkT, in_=mkT[:].rearrange("p a b -> p (a b)")), r=[mkT])
            c.dma("sp", lambda e: e.dma_start(out=d_mv, in_=mv[:].rearrange("p a b -> p (a b)")), r=[mv])

        PHASES = build.phases

        if PHASES >= 1:
          with ExitStack() as es:
            hT = sb(es, "hT", [128, 8, S], BF16)
            NW = 4
            wch = [sb(es, "wch%d" % i, [128, 8, 128], BF16) for i in range(NW)]
            wstate = {"n": 0}

            def load_chunk(ci):
                wt = wch[wstate["n"] % NW]
                wstate["n"] += 1
                load_bf16(wt, wt[:], w_fm[:, ci * 128:(ci + 1) * 128].rearrange("(kc p) n -> p kc n", p=128))
                return wt

            def proj(wt, blk, ps):
                c.mmg([(ps[:, 0:BLK], wt[:, kc, :], hT[:, kc, blk * BLK:(blk + 1) * BLK], kc == 0, kc == 7) for kc in range(8)],
                      r=[wt, hT], w=[ps])

            with ExitStack() as es1:
                wtm = sb(es1, "wtm", [128, 8, 512], BF16)
                c.op("dve", lambda e: e.memset(wtm[:], 0.0), w=[wtm])
                for kc in range(8):
                    load_bf16(wtm, wtm[:, kc, 0:264], w_tm[kc * 128:(kc + 1) * 128, :])
                xr = [sb(es1, "xr%d" % i, [128, D], F32) for i in range(2)]
                hb = [sb(es1, "hb%d" % i, [128, D], BF16) for i in range(2)]
                jk = [sb(es1, "jk%d" % i, [128, D], BF16) for i in range(2)]
                s1 = [sb(es1, "s1_%d" % i, [128, 1], F32) for i in range(2)]
                vo = [sb(es1, "vo%d" % i, [128, 256], BF16) for i in range(2)]
                wo = [sb(es1, "wo%d" % i, [128, 8], F32) for i in range(2)]
                gnm = sb(es1, "gnm", [128, D], F32)
                c.dma("sp", lambda e: e.dma_start(out=gnm[:], in_=nmix.partition_broadcast(128)), w=[gnm])

                def norm_tile(t):
                    b = t % 2
                    c.dma("sp", lambda e: e.dma_start(out=xr[b][:], in_=x[t * 128:(t + 1) * 128, :]), w=[xr[b]])
                    c.act(jk[b][:], xr[b][:], AF.Square, r=[xr[b]], w=[jk[b], s1[b]], accum_out=s1[b][:, 0:1])
                    c.act(s1[b][:], s1[b][:], AF.Sqrt, r=[s1[b], epsb], w=[s1[b]], bias=epsb[:, 0:1], scale=1.0 / D)
                    c.op("dve", lambda e: e.reciprocal(out=s1[b][:], in_=s1[b][:]), r=[s1[b]], w=[s1[b]])
                    c.stt("dve", hb[b][:], xr[b][:], s1[b][:, 0:1], gnm[:], ALU.mult, ALU.mult, r=[xr[b], s1[b], gnm], w=[hb[b]])

                norm_tile(0)
                for t in range(NT):
                    b = t % 2
                    if t + 1 < NT:
                        norm_tile(t + 1)
                    pb = PSB[t % 2]
                    c.trg([(pb.t[:, kc * 128:(kc + 1) * 128], hb[b][:, kc * 128:(kc + 1) * 128]) for kc in range(8)], ident[:],
                          r=[hb[b], ident], w=[pb])
                    c.copy("act", hT[:, :, t * 128:(t + 1) * 128], pb.t[:].rearrange("p (a b) -> p a b", b=128), r=[pb], w=[hT])
                    if "tm" in build.skip:
                        continue
                    ps = PS[t % 2]
                    c.mmg([(ps[:], hT[:, kc, t * 128:(t + 1) * 128], wtm[:, kc, :], kc == 0, kc == 7) for kc in range(8)],
                          r=[hT, wtm], w=[ps])
                    c.copy("act", vo[b][:], ps[:, 0:256], r=[ps], w=[vo[b]])
                    if "wo" not in build.skip:
                        c.ts("dve", wo[b][:], ps[:, 256:264], 8.0 ** -0.5, None, ALU.mult, None, r=[ps] + ([vo[b]] if "ser" in build.skip else []), w=[wo[b]])
                    if "vdma" not in build.skip:
                        c.dma("sp", lambda e: e.dma_start(out=s_v[t * 128:(t + 1) * 128, :], in_=vo[b][:]), r=[vo[b]])
                    if "wi" not in build.skip:
                        c.dma("sp", lambda e: e.dma_start(out=s_wi[t * 128:(t + 1) * 128, :], in_=wo[b][:]), r=[wo[b]])
                c.barrier()

            ring = {}

            def rt(esx, name, shape, dt, n=2):
                if name not in ring:
                    ring[name] = [[sb(esx, "%s_%d" % (name, i), shape, dt) for i in range(n)], 0]
                lst = ring[name]
                tl = lst[0][lst[1] % n]
                lst[1] += 1
                return tl

            if PHASES >= 2:
              with ExitStack() as es2:
                rope = sb(es2, "rope", [128, 4, S], BF16)
                for i in range(4):
                    c.dma("sp", lambda e: e.dma_start(out=rope[:, i, :], in_=ropet[i]), w=[rope])
                nrc = {"n": 0}

                def norm_rope(wts, blk, out_dram, ti, gcol, gscol, mean_off, extra_scale):
                    wa = wts[0]
                    par = nrc["n"] % 2
                    nrc["n"] += 1
                    pa, pbk = PS[2 * par], PS[2 * par + 1]
                    proj(wa, blk, pa)
                    qsb = rt(es2, "nr_qsb", [128, BLK], BF16)
                    c.copy("act", qsb[:], pa[:, 0:BLK], r=[pa], w=[qsb])
                    c.mm(pbk[:, 0:BLK], perm[:, 0 if ti == 0 else 1, :], qsb[:], True, True, r=[perm, qsb], w=[pbk])
                    sl = slice(blk * BLK, (blk + 1) * BLK)
                    t1 = rt(es2, "nr_t1", [128, BLK], F32)
                    t2 = rt(es2, "nr_t2", [128, BLK], F32)
                    ob = rt(es2, "nr_ob", [128, BLK], BF16)
                    if gcol is not None:
                        c.stt("dve", t1[:], pa[:, 0:BLK], pv_t[:, gcol:gcol + 1], rope[:, ti, sl], ALU.mult, ALU.mult,
                              r=[pa, pv_t, rope], w=[t1])
                        c.stt("dve", t2[:], pbk[:, 0:BLK], pv_t[:, gscol:gscol + 1], rope[:, ti + 1, sl], ALU.mult, ALU.mult,
                              r=[pbk, pv_t, rope], w=[t2])
                    else:
                        c.tt("dve", t1[:], pa[:, 0:BLK], rope[:, ti, sl], ALU.mult, r=[pa, rope], w=[t1])
                        c.tt("dve", t2[:], pbk[:, 0:BLK], rope[:, ti + 1, sl], ALU.mult, r=[pbk, rope], w=[t2])
                    if mean_off is not None:
                        sq = rt(es2, "nr_sq", [128, BLK], BF16)
                        c.act(sq[:], pa[:, 0:BLK], AF.Square, r=[pa], w=[sq])
                        pm = PS[4 + par]
                        c.mm(pm[:, 0:BLK], onesm[:, mean_off:mean_off + 128], sq[:], True, True, r=[onesm, sq], w=[pm])
                        rs = rt(es2, "nr_rs", [128, BLK], F32)
                        rstd_from_ms(rs[:], pm[:, 0:BLK], r=[pm, epsb], w=[rs])
                        c.tt("dve", t1[:], t1[:], t2[:], ALU.add, r=[t1, t2], w=[t1])
                        c.tt("dve", ob[:], t1[:], rs[:], ALU.mult, r=[t1, rs], w=[ob])
                    else:
                        c.stt("dve", ob[:], t1[:], extra_scale, t2[:], ALU.mult, ALU.mult, r=[t1, t2], w=[ob]) if False else None
                        c.tt("dve", t1[:], t1[:], t2[:], ALU.add, r=[t1, t2], w=[t1])
                        c.act(ob[:], t1[:], AF.Copy, r=[t1], w=[ob], scale=extra_scale)
                    c.dma("sp", lambda e: e.dma_start(out=out_dram[:, sl], in_=ob[:]), r=[ob])

                def mq_head(wts, hm, blk):
                    wa, wb = wts
                    par = nrc["n"] % 2
                    nrc["n"] += 1
                    pa, pbk, pm = PS[2 * par], PS[2 * par + 1], PS[4 + par]
                    proj(wa, blk, pa)
                    proj(wb, blk, pbk)
                    sl = slice(blk * BLK, (blk + 1) * BLK)
                    sqa = rt(es2, "nr_sq", [128, BLK], BF16)
                    sqb = rt(es2, "nr_sq", [128, BLK], BF16)
                    c.act(sqa[:], pa[:, 0:BLK], AF.Square, r=[pa], w=[sqa])
                    c.act(sqb[:], pbk[:, 0:BLK], AF.Square, r=[pbk], w=[sqb])
                    c.mm(pm[:, 0:BLK], onesm[:, 256:384], sqa[:], True, False, r=[onesm, sqa], w=[pm])
                    c.mm(pm[:, 0:BLK], onesm[:, 256:384], sqb[:], False, True, r=[onesm, sqb], w=[pm])
                    rs = rt(es2, "nr_rs", [128, BLK], F32)
                    rstd_from_ms(rs[:], pm[:, 0:BLK], r=[pm, epsb], w=[rs])
                    for cc, pp in ((0, pa), (1, pbk)):
                        ob = rt(es2, "nr_ob", [128, BLK], BF16)
                        c.stt("dve", ob[:], pp[:, 0:BLK], pv_t[:, PV_MQN + cc:PV_MQN + cc + 1], rs[:], ALU.mult, ALU.mult,
                              r=[pp, pv_t, rs], w=[ob])
                        c.dma("sp", lambda e: e.dma_start(out=s_mq[2 * hm + cc][:, sl], in_=ob[:]), r=[ob])

                def gate_chunk(wts, gi, blk):
                    wa = wts[0]
                    par = nrc["n"] % 4
                    nrc["n"] += 1
                    pa = PS[par]
                    proj(wa, blk, pa)
                    sl = slice(blk * BLK, (blk + 1) * BLK)
                    ob = rt(es2, "nr_ob", [128, BLK], BF16)
                    c.act(ob[:], pa[:, 0:BLK], AF.Sigmoid, r=[pa], w=[ob])
                    c.dma("sp", lambda e: e.dma_start(out=s_g[gi][:, sl], in_=ob[:]), r=[ob])

                jobs = []
                for h in range(8):
                    jobs.append(([C_Q + h], lambda w_, blk, h=h: norm_rope(w_, blk, s_q[h], 0, PV_QN, PV_QNS, 0, None)))
                for g in range(2):
                    jobs.append(([C_K + g], lambda w_, blk, g=g: norm_rope(w_, blk, s_k[g], 0, PV_KN, PV_KNS, 0, None)))
                for p in range(4):
                    jobs.append(([C_QI + p], lambda w_, blk, p=p: norm_rope(w_, blk, s_qi[p], 2, None, None, None, 64.0 ** -0.5)))
                jobs.append(([C_KI], lambda w_, blk: norm_rope(w_, blk, s_ki, 2, PV_IKN, PV_IKNS, 128, None)))
                for hm in range(4):
                    jobs.append(([C_MQ + 2 * hm, C_MQ + 2 * hm + 1], lambda w_, blk, hm=hm: mq_head(w_, hm, blk)))
                for gi in range(24):
                    jobs.append(([C_G + gi], lambda w_, blk, gi=gi: gate_chunk(w_, gi, blk)))
                pending = [load_chunk(ci) for ci in jobs[0][0]]
                for ji, (chs, fn) in enumerate(jobs):
                    wts = pending
                    if ji + 1 < len(jobs):
                        pending = [load_chunk(ci) for ci in jobs[ji + 1][0]]
                    for blk in range(NB):
                        fn(wts, blk)
                c.barrier()

            if PHASES >= 2:
              with ExitStack() as es3:
                TCH = min(S, 1024)
                NTC = S // TCH
                NSB = TCH // BLK
                wbd_t = sb(es3, "wbd_t", [128, 8 * 2 * 128], BF16)
                load_bf16(wbd_t, wbd_t[:], wbd)
                nsp = sb(es3, "nsp", [128, 24], F32)
                c.act(nsp[:, 0:8], pv_t[:, PV_LAM:PV_LAM + 8], AF.Exp, r=[pv_t], w=[nsp], scale=-1.0)
                c.act(nsp[:, 0:8], nsp[:, 0:8], AF.Ln, r=[nsp, epsb], w=[nsp], bias=epsb[:, 1:2], scale=1.0)
                c.ts("dve", nsp[:, 8:16], nsp[:, 0:8], -8.0, None, ALU.mult, None, r=[nsp], w=[nsp])
                c.ts("dve", nsp[:, 16:24], nsp[:, 0:8], -16.0, None, ALU.mult, None, r=[nsp], w=[nsp])
                lx = sb(es3, "lx", [128, S + 3], F32)
                lruL = [[sb(es3, "%s%d" % (nm, k_), [128, TCH], dt_) for k_ in range(2)]
                        for nm, dt_ in (("xc", F32), ("xcb", BF16), ("rr", F32), ("ii", F32), ("a2", F32), ("hh", F32),
                                        ("gx", F32), ("gt", F32), ("yb", BF16))]
                lcnt = 0
                hprev = sb(es3, "hprev", [128, 1], F32)
                c.op("dve", lambda e: e.memset(lx[:, 0:3], 0.0), w=[lx])
                pend = (load_chunk(C_LX), load_chunk(C_LG))
                for ci in range(8):
                    wa, wg = pend
                    if ci + 1 < 8:
                        pend = (load_chunk(C_LX + ci + 1), load_chunk(C_LG + ci + 1))
                    for blk in range(NB):
                        ps = PS[blk % 2]
                        proj(wa, blk, ps)
                        c.copy("act", lx[:, 3 + blk * BLK:3 + (blk + 1) * BLK], ps[:, 0:BLK], r=[ps], w=[lx])
                    for tc in range(NTC):
                        o = tc * TCH
                        xc, xcb, rr, ii, a2, hh, gx, gt, yb = [L_[lcnt % 2] for L_ in lruL]
                        lcnt += 1
                        cw = lambda j: pv_t[:, PV_CW + 8 * j + ci:PV_CW + 8 * j + ci + 1]
                        c.ts("dve", xc[:], lx[:, o:o + TCH], cw(0), pv_t[:, PV_CB + ci:PV_CB + ci + 1], ALU.mult, ALU.add,
                             r=[lx, pv_t], w=[xc])
                        for j in range(1, 4):
                            c.stt("dve", xc[:], lx[:, o + j:o + j + TCH], cw(j), xc[:], ALU.mult, ALU.add,
                                  r=[lx, pv_t, xc], w=[xc])
                        c.copy("act", xcb[:], xc[:], r=[xc], w=[xcb])
                        for sbk in range(NSB):
                            ss_ = slice(sbk * BLK, (sbk + 1) * BLK)
                            pa, pi = PS[2 + (sbk % 2) * 2], PS[3 + (sbk % 2) * 2]
                            c.mm(pa[:, 0:BLK], wbd_t[:, (ci * 2) * 128:(ci * 2 + 1) * 128], xcb[:, ss_], True, True, r=[wbd_t, xcb], w=[pa])
                            c.mm(pi[:, 0:BLK], wbd_t[:, (ci * 2 + 1) * 128:(ci * 2 + 2) * 128], xcb[:, ss_], True, True, r=[wbd_t, xcb], w=[pi])
                            c.act(rr[:, ss_], pa[:, 0:BLK], AF.Sigmoid, r=[pa, pv_t], w=[rr], bias=pv_t[:, PV_BA + ci:PV_BA + ci + 1], scale=1.0)
                            c.act(ii[:, ss_], pi[:, 0:BLK], AF.Sigmoid, r=[pi, pv_t], w=[ii], bias=pv_t[:, PV_BI + ci:PV_BI + ci + 1], scale=1.0)
                        c.act(a2[:], rr[:], AF.Exp, r=[rr, nsp], w=[a2], scale=nsp[:, 16 + ci:17 + ci])
                        c.act(rr[:], rr[:], AF.Exp, r=[rr, nsp], w=[rr], scale=nsp[:, 8 + ci:9 + ci])
                        c.act(a2[:], a2[:], AF.Sqrt, r=[a2, epsb], w=[a2], bias=epsb[:, 1:2], scale=-1.0)
                        c.tt("dve", ii[:], ii[:], xc[:], ALU.mult, r=[ii, xc], w=[ii])
                        c.tt("dve", ii[:], ii[:], a2[:], ALU.mult, r=[ii, a2], w=[ii])
                        if tc == 0:
                            c.op("dve", lambda e: e.tensor_tensor_scan(out=hh[:], data0=rr[:], data1=ii[:], initial=0.0,
                                                                       op0=ALU.mult, op1=ALU.add), r=[rr, ii], w=[hh])
                        else:
                            c.op("dve", lambda e: e.tensor_tensor_scan(out=hh[:], data0=rr[:], data1=ii[:], initial=hprev[:, 0:1],
                                                                       op0=ALU.mult, op1=ALU.add), r=[rr, ii, hprev], w=[hh])
                        if tc + 1 < NTC:
                            c.copy("dve", hprev[:], hh[:, TCH - 1:TCH], r=[hh], w=[hprev])
                        for sbk in range(NSB):
                            ss_ = slice(sbk * BLK, (sbk + 1) * BLK)
                            ps = PS[sbk % 2]
                            proj(wg, tc * NSB + sbk, ps)
                            c.copy("act", gx[:, ss_], ps[:, 0:BLK], r=[ps], w=[gx])
                            c.act(gt[:, ss_], ps[:, 0:BLK], AF.Square, r=[ps], w=[gt])
                        c.ts("dve", gt[:], gt[:], 0.044715, 1.0, ALU.mult, ALU.add, r=[gt], w=[gt])
                        c.tt("dve", gt[:], gt[:], gx[:], ALU.mult, r=[gt, gx], w=[gt])
                        c.act(gt[:], gt[:], AF.Sigmoid, r=[gt], w=[gt], scale=1.5957691216057308)
                        c.tt("dve", gt[:], gt[:], gx[:], ALU.mult, r=[gt, gx], w=[gt])
                        c.tt("dve", yb[:], gt[:], hh[:], ALU.mult, r=[gt, hh], w=[yb])
                        c.dma("sp", lambda e: e.dma_start(out=s_ylru[ci][:, o:o + TCH], in_=yb[:]), r=[yb])
                c.barrier()

        if PHASES >= 3:
          with ExitStack() as es:
            NIT = 14
            for i in range(16 if build.phases >= 5 else 0):
                c.dma("pool", lambda e: e.dma_start(out=s_uvb[i * 1024:(i + 1) * 1024, 0:D], in_=pu[i * 1024:(i + 1) * 1024, :]))
                c.dma("pool", lambda e: e.dma_start(out=s_uvb[i * 1024:(i + 1) * 1024, D:2 * D], in_=pv[i * 1024:(i + 1) * 1024, :]))
            kT = sb(es, "kT", [128, 2, S], BF16)
            vt = sb(es, "vt", [128, NT, 256], BF16)
            ki2 = sb(es, "ki2", [128, S], BF16)
            wi_t = sb(es, "wi_t", [128, NT, 8], F32)
            c.dma("sp", lambda e: e.dma_start(out=kT[:], in_=s_k.rearrange("g p t -> p g t")), w=[kT])
            c.dma("sp", lambda e: e.dma_start(out=vt[:], in_=s_v.rearrange("(t p) d -> p t d", p=128)), w=[vt])
            c.dma("sp", lambda e: e.dma_start(out=ki2[:], in_=s_ki), w=[ki2])
            c.dma("sp", lambda e: e.dma_start(out=wi_t[:], in_=s_wi.rearrange("(t p) h -> p t h", p=128)), w=[wi_t])
            cmask = sb(es, "cmask", [128, 128], F32)
            c.op("pool", lambda e: e.memset(cmask[:], 0.0), w=[cmask])
            c.op("pool", lambda e: e.affine_select(out=cmask[:], in_=cmask[:], pattern=[[-1, 128]], compare_op=ALU.is_ge,
                                                   fill=NEG, base=0, channel_multiplier=1), r=[cmask], w=[cmask])
            pw2 = sb(es, "pw2", [128, NIT], F32)
            for it in range(NIT):
                c.op("pool", lambda e: e.memset(pw2[:, it:it + 1], 2.0 ** -(it + 1)), w=[pw2])
            scd = [sb(es, "sc%d" % i, [128, S], F32) for i in range(4)]
            maskd = [sb(es, "maskb%d" % i, [128, S], BF16) for i in range(2)]
            mTd = [sb(es, "mT_sb%d" % i, [128, NT, 128], BF16) for i in range(4)]
            qT = [sb(es, "qT%d" % i, [128, 8, 128], BF16) for i in range(4)]
            qiT = [sb(es, "qiT%d" % i, [128, 4, 128], BF16) for i in range(4)]
            rl = [sb(es, "rl%d" % i, [128, 512], F32) for i in range(4)]
            Eb = [sb(es, "Eb%d" % i, [128, 512], BF16) for i in range(3)]
            Pb = [sb(es, "Pb%d" % i, [128, 512], BF16) for i in range(3)]
            std = [sb(es, "st%d" % i, [128, 8], F32) for i in range(4)]
            wkd = [sb(es, "wk%d" % i, [128, NIT], F32) for i in range(4)]
            fin_o = [sb(es, "fin_o%d" % i, [128, 512], F32) for i in range(2)]
            fin_d = [sb(es, "fin_d%d" % i, [128, 512], F32) for i in range(2)]
            yat = [sb(es, "yat%d" % i, [128, 512], BF16) for i in range(2)]
            scale_att = 128.0 ** -0.5
            cntE = {"n": 0, "f": 0}

            def idx_steps(tiles):
                T_ = []
                for i in tiles:
                    d = dict(i=i, qs=slice(i * 128, (i + 1) * 128), nk=128 * (i + 1), sc=scd[i % 4], st=std[i % 4], wk=wkd[i % 4],
                             maskb=maskd[i % 2], mT=mTd[i % 4], q=qT[i % 4], qi=qiT[i % 4], junk=maskd[i % 2], ps=PS[i % 2],
                             rl=(rl[(i % 2) * 2], rl[(i % 2) * 2 + 1]))
                    T_.append(d)
                    c.dma("sp", lambda e: e.dma_start(out=d["q"][:], in_=s_q[:, :, d["qs"]].rearrange("h p t -> p h t")), w=[d["q"]])
                    c.dma("sp", lambda e: e.dma_start(out=d["qi"][:], in_=s_qi[:, :, d["qs"]].rearrange("h p t -> p h t")), w=[d["qi"]])
                nkb_max = max((d["nk"] + 511) // 512 for d in T_)
                for kb in range(nkb_max):
                    for h in range(8):
                        for d in T_:
                            if kb * 512 >= d["nk"]:
                                continue
                            ksz = min(512, d["nk"] - kb * 512)
                            ks = slice(kb * 512, kb * 512 + ksz)
                            pp, sc, i = d["ps"], d["sc"], d["i"]
                            lo_, hi_ = (h % 2) * 64, (h % 2) * 64 + 64
                            c.mm(pp[:, 0:ksz], d["qi"][lo_:hi_, h // 2, :], ki2[lo_:hi_, ks], True, True, r=[d["qi"], ki2], w=[pp])
                            r_ = d["rl"][h % 2]
                            c.act(r_[:, 0:ksz], pp[:, 0:ksz], AF.Relu, r=[pp], w=[r_])
                            if h == 0:
                                c.ts("dve", sc[:, ks], r_[:, 0:ksz], wi_t[:, i, 0:1], None, ALU.mult, None, r=[r_, wi_t], w=[sc])
                            else:
                                c.stt("dve", sc[:, ks], r_[:, 0:ksz], wi_t[:, i, h:h + 1], sc[:, ks], ALU.mult, ALU.add,
                                      r=[r_, wi_t, sc], w=[sc])
                        yield
                for d in T_:
                    sc, st, nk = d["sc"], d["st"], d["nk"]
                    c.op("dve", lambda e: e.tensor_reduce(out=st[:, 0:1], in_=sc[:, 0:nk], axis=AX.X, op=ALU.min), r=[sc], w=[st])
                for d in T_:
                    sc, st, nk = d["sc"], d["st"], d["nk"]
                    c.op("dve", lambda e: e.tensor_reduce(out=st[:, 1:2], in_=sc[:, 0:nk], axis=AX.X, op=ALU.max), r=[sc], w=[st])
                for d in T_:
                    c.tt("dve", d["sc"][:, d["qs"]], d["sc"][:, d["qs"]], cmask[:], ALU.add, r=[d["sc"], cmask], w=[d["sc"]])
                for d in T_:
                    st = d["st"]
                    c.tt("dve", st[:, 1:2], st[:, 1:2], st[:, 0:1], ALU.subtract, r=[st], w=[st])
                for d in T_:
                    c.ts("dve", d["wk"][:], pw2[:], d["st"][:, 1:2], None, ALU.mult, None, r=[pw2, d["st"]], w=[d["wk"]])
                yield
                for it in range(NIT):
                    for d in T_:
                        st, wk = d["st"], d["wk"]
                        c.tt("dve", st[:, 2:3], st[:, 0:1], wk[:, it:it + 1], ALU.add, r=[st, wk], w=[st])
                    for d in T_:
                        st, sc, nk = d["st"], d["sc"], d["nk"]
                        c.ts("dve", d["junk"][:, 0:nk], sc[:, 0:nk], st[:, 2:3], 0.0, ALU.is_ge, ALU.add, r=[sc, st], w=[d["junk"], st],
                             accum_out=st[:, 3:4])
                    for d in T_:
                        st, wk = d["st"], d["wk"]
                        c.ts("dve", st[:, 4:5], st[:, 3:4], TOPK - 0.5, wk[:, it:it + 1], ALU.is_ge, ALU.mult, r=[st, wk], w=[st])
                    for d in T_:
                        st = d["st"]
                        c.tt("dve", st[:, 0:1], st[:, 0:1], st[:, 4:5], ALU.add, r=[st], w=[st])
                    yield
                for d in T_:
                    c.ts("dve", d["maskb"][:, 0:d["nk"]], d["sc"][:, 0:d["nk"]], d["st"][:, 0:1], None, ALU.is_ge, None,
                         r=[d["sc"], d["st"]], w=[d["maskb"]])
                for d in T_:
                    i, maskb, mT_sb = d["i"], d["maskb"], d["mT"]
                    for j0 in range(0, i + 1, 8):
                        nj = min(8, i + 1 - j0)
                        pb = PSB[(j0 // 8) % 2]
                        c.trg([(pb.t[:, (j - j0) * 128:(j - j0 + 1) * 128], maskb[:, j * 128:(j + 1) * 128]) for j in range(j0, j0 + nj)],
                              ident[:], r=[maskb, ident], w=[pb])
                        c.copy("act", mT_sb[:, j0:j0 + nj, :], pb.t[:, 0:nj * 128].rearrange("p (a b) -> p a b", b=128),
                               r=[pb], w=[mT_sb])
                        yield

            def att_steps(i):
                qs = slice(i * 128, (i + 1) * 128)
                mT_sb, q_t = mTd[i % 4], qT[i % 4]
                for g in range(2):
                    po, pd = PS[4], PS[5]
                    qg = q_t[:, 4 * g:4 * g + 4, :].rearrange("p a b -> p (a b)")

                    def st_mm(j):
                        pS = PS[2 + j % 2]
                        c.mm(pS[:], kT[:, g, j * 128:(j + 1) * 128], qg, True, True, r=[kT, q_t], w=[pS])

                    EP = {}

                    def ex(j):
                        E_, P_ = Eb[cntE["n"] % 3], Pb[cntE["n"] % 3]
                        cntE["n"] += 1
                        EP[j] = (E_, P_)
                        pS = PS[2 + j % 2]
                        c.act(E_[:], pS[:], AF.Exp, r=[pS], w=[E_], scale=scale_att)

                    st_mm(0)
                    ex(0)
                    if i >= 1:
                        st_mm(1)
                    for j in range(i + 1):
                        E_, P_ = EP.pop(j)
                        c.tt("pool", P_[:].rearrange("p (a b) -> p a b", b=128),
                             E_[:].rearrange("p (a b) -> p a b", b=128),
                             mT_sb[:, j:j + 1, :].to_broadcast([128, 4, 128]), ALU.mult, r=[E_, mT_sb], w=[P_])
                        if j + 1 <= i:
                            ex(j + 1)
                        if j + 2 <= i:
                            st_mm(j + 2)
                        c.mmg([(po[:], vt[:, j, g * 128:(g + 1) * 128], P_[:], j == 0, j == i),
                               (pd[:], ones[:], P_[:], j == 0, j == i)], r=[vt, ones, P_], w=[po, pd])
                        yield
                    fo, fd = fin_o[cntE["f"] % 2], fin_d[cntE["f"] % 2]
                    cntE["f"] += 1
                    c.copy("act", fd[:], pd[:], r=[pd], w=[fd])
                    c.copy("act", fo[:], po[:], r=[po], w=[fo])
                    c.op("dve", lambda e: e.reciprocal(out=fd[:], in_=fd[:]), r=[fd], w=[fd])
                    ya = yat[g]
                    c.tt("dve", ya[:], fo[:], fd[:], ALU.mult, r=[fo, fd], w=[ya])
                    c.dma("sp", lambda e: e.dma_start(out=s_yatt[4 * g:4 * g + 4, :, qs].rearrange("h p t -> p h t"),
                                                      in_=ya[:].rearrange("p (a b) -> p a b", b=128)), r=[ya])
                    yield

            def n_idx(tiles):
                nkb = (128 * (max(tiles) + 1) + 511) // 512
                return nkb * 8 + 1 + NIT + sum((i + 8) // 8 for i in tiles)

            def att_pair(tiles):
                for i in tiles:
                    for _ in att_steps(i):
                        yield

            def drain(g):
                for _ in g:
                    pass

            pairs = [list(range(p, min(p + 2, NT))) for p in range(0, NT, 2)]
            drain(idx_steps(pairs[0]))
            for pi, tiles in enumerate(pairs):
                ga = att_pair(tiles)
                if pi + 1 < len(pairs):
                    gi = idx_steps(pairs[pi + 1])
                    na, ni = sum(2 * (i + 2) for i in tiles), n_idx(pairs[pi + 1])
                    da = di = 0
                    fa = fi = False
                    while not (fa and fi):
                        if not fi and (fa or di * na <= da * ni):
                            try:
                                next(gi)
                                di += 1
                            except StopIteration:
                                fi = True
                        else:
                            try:
                                next(ga)
                                da += 1
                            except StopIteration:
                                fa = True
                else:
                    drain(ga)
            c.barrier()

        if PHASES >= 4:
          with ExitStack() as es:
            wout = sb(es, "wout", [128, 8, D], BF16)
            for kc in range(8):
                load_bf16(wout, wout[:, kc, :], w_out[kc * 128:(kc + 1) * 128, :])
            mqT = [sb(es, "mqT%d" % i, [128, 8, BLK], BF16) for i in range(2)]
            mrg = [sb(es, "mrg%d" % i, [128, 8, BLK], BF16) for i in range(2)]
            Em = [sb(es, "Em%d" % i, [128, BLK], BF16) for i in range(4)]
            gl = [sb(es, "gl%d" % i, [128, 5, BLK], BF16) for i in range(4)]
            ym = [sb(es, "ym%d" % i, [128, BLK], F32) for i in range(4)]
            m1 = [sb(es, "m1_%d" % i, [128, BLK], F32) for i in range(4)]
            m2 = [sb(es, "m2_%d" % i, [128, BLK], F32) for i in range(4)]
            rdm = [sb(es, "rdm%d" % i, [128, BLK], F32) for i in range(2)]
            xt = [sb(es, "xt%d" % i, [128, D], F32) for i in range(2)]
            x1t = [sb(es, "x1t%d" % i, [128, D], F32) for i in range(2)]
            scale_mem = 256.0 ** -0.5
            ncnt = 0
            for blk in range(NB):
                sl = slice(blk * BLK, (blk + 1) * BLK)
                mq_ = mqT[blk % 2]
                mg_ = mrg[blk % 2]
                c.dma("sp", lambda e: e.dma_start(out=mq_[:], in_=s_mq[:, :, sl].rearrange("h p t -> p h t")), w=[mq_])
                for hm in range(4):
                    for mt in range(2):
                        pS = PS[mt]
                        c.mmg([(pS[:, 0:BLK], mkT[:, 2 * hm + cc, mt * 128:(mt + 1) * 128], mq_[:, 2 * hm + cc, :], cc == 0, cc == 1)
                               for cc in range(2)], r=[mkT, mq_], w=[pS])
                        c.act(Em[(hm % 2) * 2 + mt][:], pS[:, 0:BLK], AF.Exp, r=[pS], w=[Em[(hm % 2) * 2 + mt]], scale=scale_mem)
                    pd = PS[4]
                    for mt in range(2):
                        c.mm(pd[:, 0:BLK], ones[:], Em[(hm % 2) * 2 + mt][:], mt == 0, mt == 1, r=[ones, Em[(hm % 2) * 2 + mt]], w=[pd])
                    rd = rdm[hm % 2]
                    c.op("dve", lambda e: e.reciprocal(out=rd[:], in_=pd[:, 0:BLK]), r=[pd], w=[rd])
                    for cc in range(2):
                        ch = 2 * hm + cc
                        po = PS[2 + cc]
                        c.mmg([(po[:, 0:BLK], mv[:, mt, hm * 256 + cc * 128:hm * 256 + (cc + 1) * 128], Em[(hm % 2) * 2 + mt][:],
                                mt == 0, mt == 1) for mt in range(2)], r=[mv, Em[(hm % 2) * 2], Em[(hm % 2) * 2 + 1]], w=[po])
                        g_ = gl[ncnt % 4]
                        y_, a_, b_ = ym[ncnt % 4], m1[ncnt % 4], m2[ncnt % 4]
                        ncnt += 1
                        for k_, src in enumerate((s_g[ch], s_g[8 + ch], s_g[16 + ch], s_ylru[ch], s_yatt[ch])):
                            c.dma("sp", lambda e: e.dma_start(out=g_[:, k_, :], in_=src[:, sl]), w=[g_])
                        c.tt("dve", y_[:], po[:, 0:BLK], rd[:], ALU.mult, r=[po, rd], w=[y_])
                        c.tt("pool", y_[:], y_[:], g_[:, 2, :], ALU.mult, r=[y_, g_], w=[y_])
                        c.tt("pool", a_[:], g_[:, 0, :], g_[:, 3, :], ALU.mult, r=[g_], w=[a_])
                        c.tt("dve", b_[:], g_[:, 1, :], g_[:, 4, :], ALU.mult, r=[g_], w=[b_])
                        c.tt("pool", a_[:], a_[:], b_[:], ALU.add, r=[a_, b_], w=[a_])
                        c.tt("dve", mg_[:, ch, :], a_[:], y_[:], ALU.add, r=[a_, y_], w=[mg_])
                for tt_ in range(BLK // 128):
                    t = blk * (BLK // 128) + tt_
                    xx, x1_ = xt[t % 2], x1t[t % 2]
                    c.dma("sp", lambda e: e.dma_start(out=xx[:], in_=x[t * 128:(t + 1) * 128, :]), w=[xx])
                    for half in range(2):
                        pO = PS[half]
                        c.mmg([(pO[:], mg_[:, ch, tt_ * 128:(tt_ + 1) * 128], wout[:, ch, half * 512:(half + 1) * 512], ch == 0, ch == 7)
                               for ch in range(8)], r=[mg_, wout], w=[pO])
                        c.tt("dve", x1_[:, half * 512:(half + 1) * 512], pO[:], xx[:, half * 512:(half + 1) * 512], ALU.add,
                             r=[pO, xx], w=[x1_])
                    c.dma("sp", lambda e: e.dma_start(out=s_x1[t * 128:(t + 1) * 128, :], in_=x1_[:]), r=[x1_])
            c.barrier()

        if PHASES >= 5:
          with ExitStack() as es:
            wq_t = sb(es, "wq_t", [128, 8, 2048], BF16)
            for kc in range(8):
                load_bf16(wq_t, wq_t[:, kc, :], w_q[kc * 128:(kc + 1) * 128, :])
            sk_t = sb(es, "sk_t", [128, 16, 128], BF16)
            load_bf16(sk_t, sk_t[:].rearrange("p a b -> p (a b)"), skT)
            gff = sb(es, "gff", [128, D], F32)
            c.dma("sp", lambda e: e.dma_start(out=gff[:], in_=nffn.partition_broadcast(128)), w=[gff])
            iota16 = sb(es, "iota16", [128, 16], F32)
            for k_ in range(16):
                c.op("pool", lambda e: e.memset(iota16[:, k_:k_ + 1], float(k_)), w=[iota16])
            NUV, NDG = 20, 4
            UVr = [sb(es, "UVr%d" % i, [128, 2 * D], BF16) for i in range(NUV)]
            dg = [sb(es, "dg%d" % i, [128, 128], BF16) for i in range(NDG)]
            junkU = sb(es, "junkU", [128, D], BF16)
            x1r = [sb(es, "x1r%d" % i, [128, D], F32) for i in range(2)]
            qtm = sb(es, "qtm", [128, 2048], BF16)
            jk5 = qtm
            r5 = [sb(es, "r5_%d" % i, [128, 1], F32) for i in range(2)]
            xnb = [sb(es, "xnb%d" % i, [128, D], BF16) for i in range(2)]
            xnT = [sb(es, "xnT%d" % i, [128, 8, 128], BF16) for i in range(2)]
            qTb = [sb(es, "qTb%d" % i, [128, 16, 128], BF16) for i in range(1)] * 2
            scs = [sb(es, "scs%d" % i, [128, 16, 128], F32) for i in range(1)] * 2
            scw = [sb(es, "scw%d" % i, [128, 128], F32) for i in range(2)]
            s16 = [sb(es, "s16_%d" % i, [128, 16, 16], F32) for i in range(2)]
            i16 = [sb(es, "i16_%d" % i, [128, 16, 16], U32) for i in range(2)]
            i16f = [sb(es, "i16f%d" % i, [128, 16, 16], F32) for i in range(2)]
            cand = [sb(es, "cand%d" % i, [128, 8, 256], F32) for i in range(1)] * 2
            cwk = [sb(es, "cwk%d" % i, [128, 256], F32) for i in range(2)]
            tops = [sb(es, "tops%d" % i, [128, 8, 16], F32) for i in range(2)]
            posu = [sb(es, "posu%d" % i, [128, 8, 16], U32) for i in range(2)]
            pab = [sb(es, "pab%d" % i, [128, 2, 128], U32) for i in range(2)]
            pabf = [sb(es, "pabf%d" % i, [128, 2, 128], F32) for i in range(2)]
            eq = [sb(es, "eq%d" % i, [128, 8, 16, 16], F32) for i in range(1)] * 2
            isel = [sb(es, "isel%d" % i, [128, 2, 128], F32) for i in range(2)]
            eidf = [sb(es, "eidf%d" % i, [128, 128], F32) for i in range(2)]
            eidx = [sb(es, "eidx%d" % i, [128, 128], I32) for i in range(2)]
            gsm = [sb(es, "gsm%d" % i, [128, 8, 16], F32) for i in range(2)]
            ssum = [sb(es, "ssum%d" % i, [128, 8], F32) for i in range(2)]
            scr = [sb(es, "scr%d" % i, [128, 128], F32) for i in range(2)]
            ga = [sb(es, "ga%d" % i, [128, 16], F32) for i in range(2)]
            gb = [sb(es, "gb%d" % i, [128, 16], F32) for i in range(2)]
            actw = [sb(es, "actw%d" % i, [128, 16], F32) for i in range(2)]
            yo = [sb(es, "yo%d" % i, [128, D], F32) for i in range(1)] * 2
            cnt5 = {"uv": 0, "d": 0, "h": 0}

            def peer_select(t):
                b = t % 2
                x1_ = x1r[b]
                c.dma("sp", lambda e: e.dma_start(out=x1_[:], in_=s_x1[t * 128:(t + 1) * 128, :]), w=[x1_])
                yield
                c.act(jk5[:, 0:D], x1_[:], AF.Square, r=[x1_], w=[jk5, r5[b]], accum_out=r5[b][:, 0:1])
                c.act(r5[b][:], r5[b][:], AF.Sqrt, r=[r5[b], epsb], w=[r5[b]], bias=epsb[:, 0:1], scale=1.0 / D)
                yield
                c.op("dve", lambda e: e.reciprocal(out=r5[b][:], in_=r5[b][:]), r=[r5[b]], w=[r5[b]])
                c.stt("dve", xnb[b][:], x1_[:], r5[b][:, 0:1], gff[:], ALU.mult, ALU.mult, r=[x1_, r5[b], gff], w=[xnb[b]])
                yield
                pb = PSB[t % 2]
                c.trg([(pb.t[:, kc * 128:(kc + 1) * 128], xnb[b][:, kc * 128:(kc + 1) * 128]) for kc in range(8)], ident[:],
                      r=[xnb[b], ident], w=[pb])
                yield
                c.copy("act", xnT[b][:], pb.t[:].rearrange("p (a b) -> p a b", b=128), r=[pb], w=[xnT[b]])
                yield
                for cg in range(5):
                    if cg < 4:
                        ps = PS[cg % 2]
                        c.mmg([(ps[:], xnT[b][:, kc, :], wq_t[:, kc, cg * 512:(cg + 1) * 512], kc == 0, kc == 7) for kc in range(8)],
                              r=[xnT[b], wq_t], w=[ps])
                    if cg >= 1:
                        ps = PS[(cg - 1) % 2]
                        c.copy("act", qtm[:, (cg - 1) * 512:cg * 512], ps[:], r=[ps], w=[qtm])
                    yield
                for g8 in range(3):
                    if g8 < 2:
                        pbq = PSB[(t + 1 + g8) % 2]
                        c.trg([(pbq.t[:, j * 128:(j + 1) * 128], qtm[:, (g8 * 8 + j) * 128:(g8 * 8 + j + 1) * 128]) for j in range(8)],
                              ident[:], r=[qtm, ident], w=[pbq])
                    if g8 >= 1:
                        pbq = PSB[(t + g8) % 2]
                        c.copy("act", qTb[b][:, (g8 - 1) * 8:g8 * 8, :], pbq.t[:].rearrange("p (a b) -> p a b", b=128), r=[pbq], w=[qTb[b]])
                    yield
                for g4 in range(5):
                    if g4 < 4:
                        ps = PS[2 + g4 % 2]
                        c.mmg([(ps[:, j * 128:(j + 1) * 128], qTb[b][:, g4 * 4 + j, :], sk_t[:, g4 * 4 + j, :], True, True) for j in range(4)],
                              r=[qTb[b], sk_t], w=[ps])
                    if g4 >= 1:
                        ps = PS[2 + (g4 - 1) % 2]
                        c.copy("act", scs[b][:, (g4 - 1) * 4:g4 * 4, :], ps[:].rearrange("p (a b) -> p a b", b=128), r=[ps], w=[scs[b]])
                    yield
                for hp in range(16):
                    w_ = scw[hp % 2]
                    c.op("dve", lambda e: e.max(out=s16[b][:, hp, 0:8], in_=scs[b][:, hp, :]), r=[scs[b]], w=[s16[b]])
                    c.op("dve", lambda e: e.match_replace(out=w_[:], in_to_replace=s16[b][:, hp, 0:8], in_values=scs[b][:, hp, :],
                                                          imm_value=NEG), r=[s16[b], scs[b]], w=[w_])
                    c.op("dve", lambda e: e.max(out=s16[b][:, hp, 8:16], in_=w_[:]), r=[w_], w=[s16[b]])
                    c.op("dve", lambda e: e.max_index(out=i16[b][:, hp, 0:8], in_max=s16[b][:, hp, 0:8], in_values=scs[b][:, hp, :]),
                         r=[s16[b], scs[b]], w=[i16[b]])
                    c.op("dve", lambda e: e.max_index(out=i16[b][:, hp, 8:16], in_max=s16[b][:, hp, 8:16], in_values=scs[b][:, hp, :]),
                         r=[s16[b], scs[b]], w=[i16[b]])
                    yield
                c.copy("dve", i16f[b][:], i16[b][:], r=[i16[b]], w=[i16f[b]])
                s16v = s16[b][:].rearrange("p (h two) k -> p h two k", two=2)
                yield
                yield
                c.tt("pool", cand[b][:].rearrange("p h (a b) -> p h a b", b=16),
                     s16v[:, :, 0, :].unsqueeze(3).to_broadcast([128, 8, 16, 16]),
                     s16v[:, :, 1, :].unsqueeze(2).to_broadcast([128, 8, 16, 16]), ALU.add, r=[s16[b]], w=[cand[b]])
                yield
                yield
                yield
                for h in range(8):
                    w_ = cwk[h % 2]
                    c.op("dve", lambda e: e.max(out=tops[b][:, h, 0:8], in_=cand[b][:, h, :]), r=[cand[b]], w=[tops[b]])
                    c.op("dve", lambda e: e.match_replace(out=w_[:], in_to_replace=tops[b][:, h, 0:8], in_values=cand[b][:, h, :],
                                                          imm_value=NEG), r=[tops[b], cand[b]], w=[w_])
                    c.op("dve", lambda e: e.max(out=tops[b][:, h, 8:16], in_=w_[:]), r=[w_], w=[tops[b]])
                    c.op("dve", lambda e: e.max_index(out=posu[b][:, h, 0:8], in_max=tops[b][:, h, 0:8], in_values=cand[b][:, h, :]),
                         r=[tops[b], cand[b]], w=[posu[b]])
                    c.op("dve", lambda e: e.max_index(out=posu[b][:, h, 8:16], in_max=tops[b][:, h, 8:16], in_values=cand[b][:, h, :]),
                         r=[tops[b], cand[b]], w=[posu[b]])
                    yield
                pflat = posu[b][:].rearrange("p h k -> p (h k)")
                c.op("dve", lambda e: e.tensor_single_scalar(out=pab[b][:, 0, :], in_=pflat, scalar=4, op=ALU.logical_shift_right),
                     r=[posu[b]], w=[pab[b]])
                c.op("dve", lambda e: e.tensor_single_scalar(out=pab[b][:, 1, :], in_=pflat, scalar=15, op=ALU.bitwise_and),
                     r=[posu[b]], w=[pab[b]])
                c.copy("dve", pabf[b][:], pab[b][:], r=[pab[b]], w=[pabf[b]])
                i16v = i16f[b][:].rearrange("p (h two) k -> p h two k", two=2)
                for side in range(2):
                    sel = pabf[b][:, side, :].rearrange("p (h k) -> p h k", k=16)
                    c.tt("dve", eq[b][:], sel.unsqueeze(3).to_broadcast([128, 8, 16, 16]),
                         iota16[:].unsqueeze(1).unsqueeze(1).to_broadcast([128, 8, 16, 16]), ALU.is_equal,
                         r=[pabf[b], iota16], w=[eq[b]])
                    yield
                    yield
                    c.tt("pool", eq[b][:], eq[b][:], i16v[:, :, side, :].unsqueeze(2).to_broadcast([128, 8, 16, 16]), ALU.mult,
                         r=[eq[b], i16f[b]], w=[eq[b]])
                    yield
                    yield
                    yield
                    c.op("dve", lambda e: e.tensor_reduce(out=isel[b][:, side, :], in_=eq[b][:].rearrange("p h k a -> p (h k) a"),
                                                          axis=AX.X, op=ALU.add), r=[eq[b]], w=[isel[b]])
                    yield
                c.stt("dve", eidf[b][:], isel[b][:, 0, :], 128.0, isel[b][:, 1, :], ALU.mult, ALU.add, r=[isel[b]], w=[eidf[b]])
                c.copy("dve", eidx[b][:], eidf[b][:], r=[eidf[b]], w=[eidx[b]])
                c.tt("dve", gsm[b][:], tops[b][:], tops[b][:, :, 0:1].to_broadcast([128, 8, 16]), ALU.subtract, r=[tops[b]], w=[gsm[b]])
                c.act(gsm[b][:], gsm[b][:], AF.Exp, r=[gsm[b]], w=[gsm[b]])
                c.op("dve", lambda e: e.tensor_reduce(out=ssum[b][:], in_=gsm[b][:], axis=AX.X, op=ALU.add), r=[gsm[b]], w=[ssum[b]])
                c.op("dve", lambda e: e.reciprocal(out=ssum[b][:], in_=ssum[b][:]), r=[ssum[b]], w=[ssum[b]])
                c.tt("dve", gsm[b][:], gsm[b][:], ssum[b][:].unsqueeze(2).to_broadcast([128, 8, 16]), ALU.mult, r=[gsm[b], ssum[b]], w=[gsm[b]])

            def peer_compute(t, selgen):
                b = t % 2
                x1_ = x1r[b]
                acc = (PS[4], PS[5])

                def dots(hd, k0, k1, vts):
                    for k_ in range(k0, k1):
                        slot = hd * 16 + k_
                        UV_ = UVr[cnt5["uv"] % NUV]
                        cnt5["uv"] += 1
                        vts.append(UV_)
                        c.dma("pool", lambda e: e.indirect_dma_start(out=UV_[:], out_offset=None, in_=s_uvb,
                                                                     in_offset=bass.IndirectOffsetOnAxis(ap=eidx[b][:, slot:slot + 1], axis=0)),
                              r=[eidx[b]], w=[UV_])
                        c.stt("dve", junkU[:], UV_[:, 0:D], 1.0, xnb[b][:], ALU.mult, ALU.mult, r=[UV_, xnb[b]], w=[junkU, scr[b]],
                              accum_out=scr[b][:, slot:slot + 1])
                        if selgen is not None and ((slot < 36 and slot % 2 == 0) or (slot >= 40 and slot % 2 == 0)):
                            next(selgen, None)

                BS = 8
                NBT = 128 // BS
                vnext = []
                dots(0, 0, BS, vnext)
                for bt in range(NBT):
                    vts = vnext
                    vnext = []
                    hb_ = cnt5["h"] % 2
                    cnt5["h"] += 1
                    s0 = bt * BS
                    sx = scr[b][:, s0:s0 + BS]
                    gsl = gsm[b][:].rearrange("p h k -> p (h k)")[:, s0:s0 + BS]
                    nhd, nk0 = (s0 + BS) // 16, (s0 + BS) % 16
                    if bt + 1 < NBT:
                        dots(nhd, nk0, nk0 + 2, vnext)
                    c.stt("dve", ga[hb_][:, 0:BS], sx, 0.044715, sx, ALU.mult, ALU.mult, r=[scr[b]], w=[ga[hb_]])
                    c.stt("dve", ga[hb_][:, 0:BS], ga[hb_][:, 0:BS], 1.0, sx, ALU.add, ALU.mult, r=[ga[hb_], scr[b]], w=[ga[hb_]])
                    c.act(gb[hb_][:, 0:BS], ga[hb_][:, 0:BS], AF.Sigmoid, r=[ga[hb_]], w=[gb[hb_]], scale=1.5957691216057308)
                    c.tt("dve", actw[hb_][:, 0:BS], sx, gsl, ALU.mult, r=[scr[b], gsm[b]], w=[actw[hb_]])
                    if bt + 1 < NBT:
                        dots(nhd, nk0 + 2, nk0 + 4, vnext)
                    c.tt("dve", actw[hb_][:, 0:BS], actw[hb_][:, 0:BS], gb[hb_][:, 0:BS], ALU.mult, r=[gb[hb_], actw[hb_]], w=[actw[hb_]])
                    for k_ in range(BS):
                        slot = s0 + k_
                        d_ = dg[cnt5["d"] % NDG]
                        cnt5["d"] += 1
                        c.act(d_[:], identf[:], AF.Copy, r=[identf, actw[hb_]], w=[d_], scale=actw[hb_][:, k_:k_ + 1])
                        c.mmg([(acc[half][:], d_[:], vts[k_][:, D + half * 512:D + (half + 1) * 512], slot == 0, slot == 127)
                               for half in range(2)], r=[d_, vts[k_]], w=[acc[0], acc[1]])
                    if bt + 1 < NBT:
                        dots(nhd, nk0 + 4, nk0 + BS, vnext)
                if selgen is not None:
                    for _ in selgen:
                        pass
                for half in range(2):
                    c.tt("dve", yo[b][:, half * 512:(half + 1) * 512], acc[half][:], x1_[:, half * 512:(half + 1) * 512], ALU.add,
                         r=[acc[half], x1_], w=[yo[b]])
                c.dma("sp", lambda e: e.dma_start(out=y[t * 128:(t + 1) * 128, :], in_=yo[b][:]), r=[yo[b]])

            for _ in peer_select(0):
                pass
            for t in range(NT):
                peer_compute(t, peer_select(t + 1) if t + 1 < NT else None)
            c.barrier()

        c.barrier(engines=("sp",))
    return nc


build.phases = 99
build.skip = set()


def rope_tables(S):
    pos = np.arange(S, dtype=np.float32)
    out = np.zeros((4, 128, S), np.float32)
    for ti, hd in ((0, 128), (2, 64)):
        half = hd // 2
        freq = (np.float32(10000.0) ** (-np.arange(half, dtype=np.float32) / np.float32(half))).astype(np.float32)
        ang = pos[None, :] * freq[:, None]
        cs, sn = np.cos(ang).astype(np.float32), np.sin(ang).astype(np.float32)
        for p in range(128):
            d = p % hd
            j = d % half
            out[ti, p] = cs[j]
            out[ti + 1, p] = -sn[j] if d < half else sn[j]
    return out.astype(ml_dtypes.bfloat16)


def _swap_cols(w, hd):
    n = w.shape[1] // hd
    w = w.reshape(w.shape[0], n, 2, hd // 2)
    return np.ascontiguousarray(w[:, :, ::-1, :]).reshape(w.shape[0], n * hd)


def host_layout(inp, S):
    w_in = np.asarray(inp["w_in"][0], np.float32)
    sp = np.cumsum([0, 1024, 1024, 1024, 256, 256, 512, 64, 8, 1024, 3072])
    lx, lg, q, k, v, qi, ki, wi, mq, gates = [w_in[:, sp[i]:sp[i + 1]] for i in range(10)]
    ki2 = np.concatenate([ki, ki], axis=1)
    w_fm = np.concatenate([lx, lg, q, _swap_cols(q, 128), k, _swap_cols(k, 128), qi, _swap_cols(qi, 64),
                           ki2, _swap_cols(ki2, 64), mq, gates], axis=1)
    assert w_fm.shape[1] == NCH * 128
    w_tm = np.concatenate([v, wi], axis=1)

    def fm(vec):
        return np.asarray(vec, np.float32).reshape(-1, 128).T

    def sw(vec, hd):
        vec = np.asarray(vec, np.float32)
        return np.concatenate([vec[hd // 2:], vec[:hd // 2]])

    ikn = np.asarray(inp["idx_k_norm"][0], np.float32)
    cols = [fm(inp["norm_mix"][0])]
    cols += [fm(inp["conv_w"][0][j]) for j in range(4)]
    cols += [fm(inp["conv_b"][0]), fm(inp["lru_ba"][0].reshape(-1)), fm(inp["lru_bi"][0].reshape(-1)), fm(inp["lru_lambda"][0])]
    cols += [fm(inp["q_norm"][0]), fm(sw(inp["q_norm"][0], 128)), fm(inp["k_norm"][0]), fm(sw(inp["k_norm"][0], 128))]
    cols += [fm(np.concatenate([ikn, ikn])), fm(np.concatenate([sw(ikn, 64), sw(ikn, 64)]))]
    cols += [fm(inp["mem_norm"][0]), fm(inp["mem_q_norm"][0]), fm(inp["mem_k_norm"][0]), fm(inp["norm_ffn"][0])]
    pvec = np.ascontiguousarray(np.concatenate(cols, axis=1))
    assert pvec.shape == (128, NPV), pvec.shape
    wbd = np.zeros((128, 8, 2, 128), np.float32)
    for ci in range(8):
        for j, nm in enumerate(("lru_wa", "lru_wi")):
            w = np.asarray(inp[nm][0], np.float32)
            wbd[0:64, ci, j, 0:64] = w[2 * ci]
            wbd[64:128, ci, j, 64:128] = w[2 * ci + 1]
    skT = np.ascontiguousarray(np.asarray(inp["peer_subkeys"][0], np.float32).reshape(16, 128, 128).transpose(2, 0, 1))
    shared = {
        "w_fm": np.ascontiguousarray(w_fm), "w_tm": np.ascontiguousarray(w_tm), "pvec": pvec,
        "wbd": wbd.reshape(128, -1), "ropet": rope_tables(S),
        "w_mem_kv": np.ascontiguousarray(inp["w_mem_kv"][0], np.float32),
        "w_out": np.ascontiguousarray(inp["w_out"][0], np.float32),
        "nffn": np.ascontiguousarray(inp["norm_ffn"][0], np.float32).reshape(1, D),
        "nmix": np.ascontiguousarray(inp["norm_mix"][0], np.float32).reshape(1, D),
        "w_q": np.ascontiguousarray(inp["peer_wq"][0], np.float32),
        "skT": skT.reshape(128, -1),
        "pu": np.ascontiguousarray(inp["peer_u"][0], np.float32),
        "pv": np.ascontiguousarray(inp["peer_v"][0], np.float32),
    }
    return shared


def kernel(**inputs):
    xs = np.asarray(inputs["x"], np.float32)
    mems = np.asarray(inputs["mem"], np.float32)
    B, S, _ = xs.shape
    shared = host_layout(inputs, S)
    nc = build(S)
    in_maps = []
    for b in range(B):
        m = dict(shared)
        m["x"] = np.ascontiguousarray(xs[b])
        m["mem"] = np.ascontiguousarray(mems[b])
        in_maps.append(m)
    res = run_bass_kernel_spmd(nc, in_maps, core_ids=list(range(B)))
    return np.stack([np.asarray(r["y"], np.float32) for r in res.results], axis=0)
```
